# Optimizing a Trainium2 kernel written in Bass

```python
import math
import jax, jax.numpy as jnp
from jax import lax
import numpy as np

D_MODEL = 2048
BATCH = 8
SEQ = 2048
DEPTH = 1

MEM_LEN = 256
CONV_CH = 1024
CONV_WIDTH = 31
N_Q_HEADS = 16
N_KV_HEADS = 2
HEAD_DIM = 64
ATTN_WIDTH = N_Q_HEADS * HEAD_DIM
KV_WIDTH = N_KV_HEADS * HEAD_DIM
WINDOW = 128
Q_BLOCK = WINDOW
NUM_BUCKETS = 32
MAX_DISTANCE = 128
X_HEADS = 4
X_HEAD_DIM = 128
X_WIDTH = X_HEADS * X_HEAD_DIM
N_GROUPS = 4
EXPERTS_PER_GROUP = 8
N_EXPERTS = N_GROUPS * EXPERTS_PER_GROUP
TOP_K = 2
D_EXPERT = 512
DISPATCH_BLOCK = 256
N_BRANCHES = 2
IN_WIDTH = 2 * CONV_CH + ATTN_WIDTH + 2 * KV_WIDTH + N_BRANCHES * D_MODEL
EPS = 1e-6
NEG = -1e30

kernel_name = "hybrid_conv_swa_memxattn_hmoe"


def rms_norm(x, g):
    xf = x.astype(jnp.float32)
    y = xf * lax.rsqrt(jnp.mean(xf * xf, axis=-1, keepdims=True) + EPS)
    return (y * g.astype(jnp.float32)).astype(x.dtype)


def layer_norm(x, g, b):
    xf = x.astype(jnp.float32)
    mu = jnp.mean(xf, axis=-1, keepdims=True)
    var = jnp.mean(jnp.square(xf - mu), axis=-1, keepdims=True)
    y = (xf - mu) * lax.rsqrt(var + EPS)
    return (y * g.astype(jnp.float32) + b.astype(jnp.float32)).astype(x.dtype)


def t5_bucket(dist):
    n = jnp.maximum(dist, 0)
    max_exact = NUM_BUCKETS // 2
    large = max_exact + (jnp.log(jnp.maximum(n, 1).astype(jnp.float32) / max_exact)
                         / math.log(MAX_DISTANCE / max_exact)
                         * (NUM_BUCKETS - max_exact)).astype(jnp.int32)
    large = jnp.minimum(large, NUM_BUCKETS - 1)
    return jnp.where(n < max_exact, n, large)


def conformer_conv(a, b, dw_w, dw_b, ln_g, ln_b, w_out):
    u = a * jax.nn.sigmoid(b)
    u = lax.conv_general_dilated(
        u, dw_w.astype(u.dtype), window_strides=(1,), padding=[(CONV_WIDTH - 1, 0)],
        dimension_numbers=('NWC', 'WIO', 'NWC'), feature_group_count=CONV_CH) + dw_b
    u = jax.nn.silu(layer_norm(u, ln_g, ln_b))
    return u @ w_out


def sliding_window_gqa(q, k, v, q_g, k_g, sinks, rel_bias):
    B, T = q.shape[:2]
    nb = T // Q_BLOCK
    G = N_Q_HEADS // N_KV_HEADS
    q = rms_norm(q.reshape(B, T, N_Q_HEADS, HEAD_DIM), q_g)
    q = q.reshape(B, nb, Q_BLOCK, N_KV_HEADS, G, HEAD_DIM)
    k = rms_norm(k.reshape(B, T, N_KV_HEADS, HEAD_DIM), k_g)
    v = v.reshape(B, T, N_KV_HEADS, HEAD_DIM)

    def band(t):
        tp = jnp.pad(t, ((0, 0), (Q_BLOCK, 0), (0, 0), (0, 0)))
        prev = tp[:, :T].reshape(B, nb, Q_BLOCK, N_KV_HEADS, HEAD_DIM)
        cur = t.reshape(B, nb, Q_BLOCK, N_KV_HEADS, HEAD_DIM)
        return jnp.concatenate([prev, cur], axis=2)

    kb, vb = band(k), band(v)
    qi = jnp.arange(Q_BLOCK)[:, None]
    kj = jnp.arange(2 * Q_BLOCK)[None, :]
    dist = qi + Q_BLOCK - kj
    bias = rel_bias[t5_bucket(dist)].astype(jnp.float32)
    bias = bias.transpose(2, 0, 1).reshape(N_KV_HEADS, G, Q_BLOCK, 2 * Q_BLOCK)
    key_pos = jnp.arange(nb)[:, None, None] * Q_BLOCK + kj[None] - Q_BLOCK
    valid = (dist >= 0) & (dist < WINDOW) & (key_pos >= 0)

    s = jnp.einsum('bnqkgd,bnskd->bnkgqs', q.astype(jnp.float32), kb.astype(jnp.float32))
    s = s * (HEAD_DIM ** -0.5) + bias
    s = jnp.where(valid[None, :, None, None], s, NEG)
    sink = sinks.astype(jnp.float32).reshape(N_KV_HEADS, G, 1, 1)
    m = jnp.maximum(s.max(axis=-1, keepdims=True), sink)
    p = jnp.exp(s - m)
    p = p / (p.sum(axis=-1, keepdims=True) + jnp.exp(sink - m))
    o = jnp.einsum('bnkgqs,bnskd->bnqkgd', p.astype(vb.dtype), vb)
    return o.reshape(B, T, ATTN_WIDTH)


def memory_cross_attention(xn, memn, w_xq, w_xkv, qg, kg, w_xo):
    B, T, _ = xn.shape
    M = memn.shape[1]
    q = rms_norm((xn @ w_xq).reshape(B, T, X_HEADS, X_HEAD_DIM), qg)
    kv = (memn @ w_xkv).reshape(B, M, 2, X_HEADS, X_HEAD_DIM)
    k = rms_norm(kv[:, :, 0], kg)
    v = kv[:, :, 1]
    s = jnp.einsum('bthd,bmhd->bhtm', q.astype(jnp.float32), k.astype(jnp.float32))
    p = jax.nn.softmax(s * (X_HEAD_DIM ** -0.5), axis=-1).astype(v.dtype)
    o = jnp.einsum('bhtm,bmhd->bthd', p, v).reshape(B, T, X_WIDTH)
    return o @ w_xo


def hierarchical_moe(xn, w_rg, b_rg, w_re, b_re, w_gu, w_dn):
    B, T, D = xn.shape
    xt = xn.reshape(-1, D)
    N = xt.shape[0]
    lg = (xt @ w_rg).astype(jnp.float32) + b_rg.astype(jnp.float32)
    pg_top, grp = lax.top_k(jax.nn.softmax(lg, axis=-1), 1)
    le = ((xt @ w_re).astype(jnp.float32) + b_re.astype(jnp.float32)).reshape(N, N_GROUPS, EXPERTS_PER_GROUP)
    le_sel = jnp.take_along_axis(le, grp[:, :, None], axis=1)[:, 0]
    pe_top, idx = lax.top_k(jax.nn.softmax(le_sel, axis=-1), TOP_K)
    pe_top = pe_top / pe_top.sum(axis=-1, keepdims=True)
    gate = pg_top * pe_top
    eid = grp * EXPERTS_PER_GROUP + idx

    A = N * TOP_K
    flat_e = eid.reshape(A)
    flat_tok = jnp.repeat(jnp.arange(N, dtype=jnp.int32), TOP_K)
    flat_w = gate.reshape(A)
    order = jnp.argsort(flat_e)
    se, stok, sw = flat_e[order], flat_tok[order], flat_w[order]
    counts = jnp.bincount(flat_e, length=N_EXPERTS)
    padded = (counts + DISPATCH_BLOCK - 1) // DISPATCH_BLOCK * DISPATCH_BLOCK
    pad_end = jnp.cumsum(padded)
    pad_start = pad_end - padded
    start = jnp.cumsum(counts) - counts
    dest = pad_start[se] + jnp.arange(A, dtype=jnp.int32) - start[se]
    n_blocks = -(-A // DISPATCH_BLOCK) + N_EXPERTS
    rows = n_blocks * DISPATCH_BLOCK
    row_tok = jnp.full((rows,), N, dtype=jnp.int32).at[dest].set(stok)
    x_pad = jnp.concatenate([xt, jnp.zeros((1, D), xt.dtype)], axis=0)
    xs = x_pad[row_tok].reshape(n_blocks, DISPATCH_BLOCK, D)
    blk_e = jnp.minimum(jnp.searchsorted(pad_end, jnp.arange(n_blocks) * DISPATCH_BLOCK, side='right'),
                        N_EXPERTS - 1)

    def expert_block(args):
        xb, e = args
        gu = xb @ w_gu[e]
        g, u = gu[:, :D_EXPERT], gu[:, D_EXPERT:]
        return (jax.nn.silu(g) * u) @ w_dn[e]

    ys = lax.map(expert_block, (xs, blk_e)).reshape(rows, D)
    contrib = ys[dest] * sw[:, None].astype(ys.dtype)
    out = jax.ops.segment_sum(contrib, stok, num_segments=N)
    return out.reshape(B, T, D)


def setup_inputs(seed: int = 0) -> dict:
    key = jax.random.key(seed)
    ks = jax.random.split(key, 32)
    nrm = jax.random.normal
    f32 = jnp.float32
    L, D = DEPTH, D_MODEL

    def gain(k, shape):
        return 1.0 + 0.02 * nrm(k, shape, f32)

    return {
        'x': nrm(ks[0], (BATCH, SEQ, D), f32),
        'mem': nrm(ks[1], (BATCH, MEM_LEN, D), f32),
        'rel_bias': 0.1 * nrm(ks[2], (NUM_BUCKETS, N_Q_HEADS), f32),
        'norm_mix_g': gain(ks[3], (L, D)),
        'w_in': nrm(ks[4], (L, D, IN_WIDTH), f32) * D ** -0.5,
        'conv_dw_w': nrm(ks[5], (L, CONV_WIDTH, 1, CONV_CH), f32) * CONV_WIDTH ** -0.5,
        'conv_dw_b': 0.02 * nrm(ks[6], (L, CONV_CH), f32),
        'conv_ln_g': gain(ks[7], (L, CONV_CH)),
        'conv_ln_b': 0.02 * nrm(ks[8], (L, CONV_CH), f32),
        'w_conv_out': nrm(ks[9], (L, CONV_CH, D), f32) * CONV_CH ** -0.5,
        'q_norm_g': gain(ks[10], (L, HEAD_DIM)),
        'k_norm_g': gain(ks[11], (L, HEAD_DIM)),
        'attn_sinks': 0.5 * nrm(ks[12], (L, N_Q_HEADS), f32),
        'w_attn_out': nrm(ks[13], (L, ATTN_WIDTH, D), f32) * ATTN_WIDTH ** -0.5,
        'w_mix_out': nrm(ks[14], (L, D, D), f32) * D ** -0.5,
        'norm_x_g': gain(ks[15], (L, D)),
        'norm_mem_g': gain(ks[16], (L, D)),
        'w_xq': nrm(ks[17], (L, D, X_WIDTH), f32) * D ** -0.5,
        'w_xkv': nrm(ks[18], (L, D, 2 * X_WIDTH), f32) * D ** -0.5,
        'xq_norm_g': gain(ks[19], (L, X_HEAD_DIM)),
        'xk_norm_g': gain(ks[20], (L, X_HEAD_DIM)),
        'w_xo': nrm(ks[21], (L, X_WIDTH, D), f32) * X_WIDTH ** -0.5,
        'norm_moe_g': gain(ks[22], (L, D)),
        'w_router_group': nrm(ks[23], (L, D, N_GROUPS), f32) * D ** -0.5,
        'b_router_group': 0.01 * nrm(ks[24], (L, N_GROUPS), f32),
        'w_router_expert': nrm(ks[25], (L, D, N_EXPERTS), f32) * D ** -0.5,
        'b_router_expert': 0.01 * nrm(ks[26], (L, N_EXPERTS), f32),
        'w_expert_gu': nrm(ks[27], (L, N_EXPERTS, D, 2 * D_EXPERT), f32) * D ** -0.5,
        'w_expert_down': nrm(ks[28], (L, N_EXPERTS, D_EXPERT, D), f32) * D_EXPERT ** -0.5,
    }


def reference(x, mem, rel_bias, norm_mix_g, w_in, conv_dw_w, conv_dw_b, conv_ln_g, conv_ln_b,
              w_conv_out, q_norm_g, k_norm_g, attn_sinks, w_attn_out, w_mix_out, norm_x_g,
              norm_mem_g, w_xq, w_xkv, xq_norm_g, xk_norm_g, w_xo, norm_moe_g, w_router_group,
              b_router_group, w_router_expert, b_router_expert, w_expert_gu, w_expert_down):
    B, T, D = x.shape
    splits = [CONV_CH, 2 * CONV_CH, 2 * CONV_CH + ATTN_WIDTH,
              2 * CONV_CH + ATTN_WIDTH + KV_WIDTH, 2 * CONV_CH + ATTN_WIDTH + 2 * KV_WIDTH]
    h = x
    for l in range(DEPTH):
        xn = rms_norm(h, norm_mix_g[l])
        proj = xn @ w_in[l]
        ca, cb, q, k, v, gl = jnp.split(proj, splits, axis=-1)
        y_conv = conformer_conv(ca, cb, conv_dw_w[l], conv_dw_b[l], conv_ln_g[l], conv_ln_b[l], w_conv_out[l])
        y_attn = sliding_window_gqa(q, k, v, q_norm_g[l], k_norm_g[l], attn_sinks[l], rel_bias) @ w_attn_out[l]
        g = jax.nn.sigmoid(gl.reshape(B, T, N_BRANCHES, D))
        merged = g[:, :, 0] * y_conv + g[:, :, 1] * y_attn
        h = h + merged @ w_mix_out[l]
        h = h + memory_cross_attention(rms_norm(h, norm_x_g[l]), rms_norm(mem, norm_mem_g[l]),
                                       w_xq[l], w_xkv[l], xq_norm_g[l], xk_norm_g[l], w_xo[l])
        h = h + hierarchical_moe(rms_norm(h, norm_moe_g[l]), w_router_group[l], b_router_group[l],
                                 w_router_expert[l], b_router_expert[l], w_expert_gu[l], w_expert_down[l])
    return h
```

```python
import numpy as np
from contextlib import ExitStack
import concourse.bass as bass
import concourse.mybir as mybir
from concourse.bass_utils import run_bass_kernel_spmd

F32 = mybir.dt.float32
BF16 = mybir.dt.bfloat16
I32 = mybir.dt.int32
U32 = mybir.dt.uint32
AF = mybir.ActivationFunctionType
ALU = mybir.AluOpType
AX = mybir.AxisListType

D = 2048
T = 2048
NT = T // 128
KC = D // 128
MEM = 256
CONV_CH = 1024
CW = 31
NQH = 16
NKV = 2
HD = 64
ATT = 1024
XH = 4
XHD = 128
XW = 512
NG = 4
EPG = 8
NE = 32
DE = 512
IN_W = 7424
EPS = 1e-6
CAP = 288
NEGM = -30000.0
NPRE = 32
OFF_CA, OFF_CB, OFF_Q, OFF_K, OFF_V, OFF_G = 0, 1024, 2048, 3072, 3200, 3328

SAME_ENG_SYNC = True


class Buf:
    def __init__(self, name):
        self.name = name
        self.w = None
        self.r = {}
        self.dsem = None
        self.dcnt = 0


class Prog:
    def __init__(self, nc, es):
        self.nc = nc
        self.es = ExitStack()
        self.engs = {'pe': nc.tensor, 'dve': nc.vector, 'act': nc.scalar, 'pool': nc.gpsimd, 'sp': nc.sync}
        self.esem = {k: self.es.enter_context(nc.semaphore('e_' + k)) for k in self.engs}
        self.ecnt = {k: 0 for k in self.engs}
        self.waited = {}
        self.nsem = len(self.engs)
        self.nbuf = 0
        self.dbufs = []

    def buf(self, name=None):
        self.nbuf += 1
        return Buf(name or ('b%d' % self.nbuf))

    def _wait(self, eng, tok):
        sem, val, key = tok
        if key == 'e_' + eng and (eng in ('pe', 'sp') or not SAME_ENG_SYNC):
            return
        if self.waited.get((eng, key), 0) >= val:
            return
        self.engs[eng].wait_ge(sem, val)
        self.waited[(eng, key)] = val

    def _deps(self, eng, r, w):
        for b in r:
            if b.w is not None:
                self._wait(eng, b.w)
        for b in w:
            if b.w is not None:
                self._wait(eng, b.w)
            for t in b.r.values():
                self._wait(eng, t)

    def _mark(self, tok, r, w):
        for b in r:
            old = b.r.get(tok[2])
            if old is None or old[1] < tok[1]:
                b.r[tok[2]] = tok
        for b in w:
            b.w = tok
            b.r = {}

    def op(self, eng, fn, r=(), w=(), signal=True):
        self._deps(eng, r, w)
        ins = fn(self.engs[eng])
        if signal:
            self.ecnt[eng] += 1
            ins.then_inc(self.esem[eng], 1)
            tok = (self.esem[eng], self.ecnt[eng], 'e_' + eng)
        else:
            tok = (self.esem[eng], self.ecnt[eng] + 1, 'e_' + eng)
        self._mark(tok, r, w)
        return ins

    def dma(self, q, pairs, r=(), w=(), **kw):
        self._deps(q, r, w)
        wb = w[0]
        if wb.dsem is None:
            wb.dsem = self.es.enter_context(self.nc.semaphore('d_' + wb.name))
            self.nsem += 1
        for (o, i) in pairs:
            self.engs[q].dma_start(out=o, in_=i, **kw).then_inc(wb.dsem, 16)
            wb.dcnt += 16
        tok = (wb.dsem, wb.dcnt, 'd_' + wb.name)
        if wb not in self.dbufs:
            self.dbufs.append(wb)
        self._mark(tok, r, w)

    def dma_nowait(self, q, pairs, wb):
        if wb.dsem is None:
            wb.dsem = self.es.enter_context(self.nc.semaphore('d_' + wb.name))
            self.nsem += 1
        for (o, i) in pairs:
            self.engs[q].dma_start(out=o, in_=i).then_inc(wb.dsem, 16)
            wb.dcnt += 16
        wb.w = (wb.dsem, wb.dcnt, 'd_' + wb.name)

    def dma_custom(self, q, fn, n, r=(), w=()):
        self._deps(q, r, w)
        wb = w[0]
        if wb.dsem is None:
            wb.dsem = self.es.enter_context(self.nc.semaphore('d_' + wb.name))
            self.nsem += 1
        for k in range(n):
            fn(self.engs[q], k).then_inc(wb.dsem, 16)
            wb.dcnt += 16
        tok = (wb.dsem, wb.dcnt, 'd_' + wb.name)
        if wb not in self.dbufs:
            self.dbufs.append(wb)
        self._mark(tok, r, w)

    def barrier(self):
        for e in self.engs:
            for o in self.engs:
                if o != e and self.ecnt[o] > 0:
                    self._wait(e, (self.esem[o], self.ecnt[o], 'e_' + o))
            for b in self.dbufs:
                self._wait(e, (b.dsem, b.dcnt, 'd_' + b.name))
        self.dbufs = []

    def wait_buf(self, eng, b):
        if b.w is not None:
            self._wait(eng, b.w)


class TB:
    def __init__(self, t, b):
        self.t = t
        self.b = b


class Kern:
    def __init__(self, stop_after=None, debug=()):
        self.stop_after = stop_after
        self.debug = set(debug)
        self.nc = bass.Bass("TRN2", target_bir_lowering=False)
        self.dbg_out = {}

    def sb(self, es, name, shape, dt):
        t = es.enter_context(self.nc.sbuf_tensor("s_" + name, shape, dt))
        return TB(t, self.p.buf(name))

    def ps(self, es, name, shape, dt=F32):
        t = es.enter_context(self.nc.psum_tensor("p_" + name, shape, dt))
        return TB(t, self.p.buf(name))

    def din(self, name, shape, dt=F32):
        return self.nc.dram_tensor(name, list(shape), dt, kind="ExternalInput").ap()

    def dump(self, name, tb, shape, dt=F32):
        if name not in self.debug:
            return
        o = self.nc.dram_tensor("dbg_" + name, list(shape), dt, kind="ExternalOutput").ap()
        ob = self.p.buf("dbg_" + name)
        self.p.dma('sp', [(o, tb.t[:])], r=[tb.b], w=[ob])
        self.dbg_out[name] = ob

    def load_w(self, dst, dstb, src_rows_cols, q='pool'):
        self.p.dma(q, [(dst, src_rows_cols.rearrange("(k p) c -> p k c", p=128))], w=[dstb])

    def build(self):
        nc = self.nc
        es = ExitStack()
        self.es = es
        self.p = Prog(nc, es)
        p = self.p
        I = {}
        I['x'] = self.din('x', [T, D])
        I['mem'] = self.din('mem', [MEM, D])
        I['w_in'] = self.din('w_in', [D, IN_W])
        I['w_conv_out'] = self.din('w_conv_out', [CONV_CH, D])
        I['w_attn_out'] = self.din('w_attn_out', [ATT, D])
        I['w_mix_out'] = self.din('w_mix_out', [D, D])
        I['w_xq'] = self.din('w_xq', [D, XW])
        I['w_xkv'] = self.din('w_xkv', [D, 2 * XW])
        I['w_xo'] = self.din('w_xo', [XW, D])
        I['w_gu'] = self.din('w_gu', [NE * D, 2 * DE])
        I['w_dn'] = self.din('w_dn', [NE * DE, D])
        I['w_rt'] = self.din('w_rt', [D, 36])
        I['pcol'] = self.din('pcol', [128, PCOL_N])
        I['prow'] = self.din('prow', [128, PROW_N])
        I['biasT'] = self.din('biasT', [128, NQH * 256])
        I['cst'] = self.din('cst', [128, CST_N])
        I['gmoe_row'] = self.din('gmoe_row', [128, D])
        I['zsrc'] = self.din('zsrc', [640, D // 2])
        self.I = I
        self.y = nc.dram_tensor("y", [T, D], F32, kind="ExternalOutput").ap()
        self.yb = p.buf('y')

        ces = ExitStack()
        self.ces = ces
        self.consts(ces)
        self.precast_init()
        with es:
            self.phaseA()
            if self.stop_after == 'A':
                return self.finish()
            self.phaseM()
            self.phaseB_conv()
            if self.stop_after == 'Bc':
                return self.finish()
            self.phaseB_attn()
            if self.stop_after == 'Ba':
                return self.finish()
            self.phaseC()
            if self.stop_after == 'C':
                return self.finish()
        es = ExitStack()
        self.es = es
        with es:
            self.phaseD()
            if self.stop_after == 'D':
                return self.finish()
            self.phaseR()
            if self.stop_after == 'R':
                return self.finish()
            self.phaseG()
            if self.stop_after == 'G':
                return self.finish()
            self.phaseH()
            return self.finish()
            return self.finish()

    def finish(self):
        p = self.p
        for ob in list(self.dbg_out.values()) + [self.yb]:
            if ob.w is not None:
                p._wait('sp', ob.w)
        return self.nc

    def consts(self, es):
        nc, p, I = self.nc, self.p, self.I
        self.pcol = self.sb(es, "pcol", [128, PCOL_N], F32)
        self.prow = self.sb(es, "prow", [128, PROW_N], F32)
        self.cst = self.sb(es, "cst", [128, CST_N], F32)
        p.dma('sp', [(self.pcol.t[:], I['pcol'])], w=[self.pcol.b])
        p.dma('sp', [(self.prow.t[:], I['prow'])], w=[self.prow.b])
        p.dma('sp', [(self.cst.t[:], I['cst'])], w=[self.cst.b])
        self.ident_bf = self.sb(es, "ident_bf", [128, 128], BF16)
        self.ident_f = TB(self.cst.t, self.cst.b)
        p.op('dve', lambda e: e.tensor_copy(out=self.ident_bf.t[:], in_=self.cst.t[:, CST['ident']:CST['ident'] + 128]),
             r=[self.cst.b], w=[self.ident_bf.b])
        self.ones_bf = self.sb(es, "ones_bf", [128, 128], BF16)
        p.op('dve', lambda e: e.memset(self.ones_bf.t[:], 1.0), w=[self.ones_bf.b])
        self.ones_f = self.sb(es, "ones_f", [128, 128], F32)
        p.op('dve', lambda e: e.memset(self.ones_f.t[:], 1.0), w=[self.ones_f.b])
        self.kxT = self.sb(es, "kxT", [128, XH, MEM], BF16)
        self.vx = self.sb(es, "vx", [128, 2, XW], BF16)
        self.blk_bf = self.sb(es, "blk_bf", [128, 128], BF16)
        p.op('dve', lambda e: e.tensor_copy(out=self.blk_bf.t[:], in_=self.cst.t[:, CST['blk']:CST['blk'] + 128]),
             r=[self.cst.b], w=[self.blk_bf.b])

    def precast_init(self):
        nc, p = self.nc, self.p
        self.wguB = nc.dram_tensor("wguB", [NE * D, 2 * DE], BF16, kind="Internal").ap()
        self.wdnB = nc.dram_tensor("wdnB", [NE * DE, D], BF16, kind="Internal").ap()
        self.wB_g = [p.buf("wB_g%d" % i) for i in range(4)]
        self.precast_i = 0
        NR = NE * CAP
        self.xsS = nc.dram_tensor("xsS", [NR + 1, D], BF16, kind="Internal").ap()
        self.xs_zero = p.buf("xs_zero")
        self.zf_r0 = 0

    def zero_fill_next(self, n=1, after=None):
        p = self.p
        NR = NE * CAP
        xsf = self.xsS.bitcast(F32)
        if after is not None and after.w is not None and self.zf_r0 < NR + 1:
            p._wait('sp', after.w)
        for _ in range(n):
            r0 = self.zf_r0
            if r0 >= NR + 1:
                return
            m = min(640, NR + 1 - r0)
            self.zf_r0 += m
            p.dma_nowait('sp', [(xsf[r0:r0 + m, :], self.I['zsrc'][0:m, :])], self.xs_zero)

    def precast_next(self, n=1, after=None):
        p, I = self.p, self.I
        if after is not None and after.w is not None and self.precast_i < NPRE:
            p._wait('pool', after.w)
        for _ in range(n):
            ex = self.precast_i
            if ex >= NPRE:
                return
            self.precast_i += 1
            pairs = []
            for hh in range(2):
                r0 = ex * D + hh * 1024
                pairs.append((self.wguB[r0:r0 + 1024, :].rearrange("(a b) c -> a (b c)", b=2),
                              I['w_gu'][r0:r0 + 1024, :].rearrange("(a b) c -> a (b c)", b=2)))
            pairs.append((self.wdnB[ex * DE:(ex + 1) * DE, :], I['w_dn'][ex * DE:(ex + 1) * DE, :]))
            p.dma_nowait('pool', pairs, self.wB_g[ex // 8])

    def pc(self, name, i=0, n=1):
        o = PCOL[name] + i
        return self.pcol.t[:, o:o + n]

    def rms_stats(self, src, srcb, junk, ssq, sd, rstd):
        p = self.p
        p.op('act', lambda e: e.activation(out=junk.t[:], in_=src, func=AF.Square, accum_out=ssq.t[:]),
             r=[srcb], w=[junk.b, ssq.b])
        p.op('act', lambda e: e.activation(out=sd.t[:], in_=ssq.t[:], func=AF.Sqrt, bias=self.epsc(), scale=1.0 / D),
             r=[ssq.b, self.cst.b], w=[sd.b])
        p.op('dve', lambda e: e.reciprocal(out=rstd.t[:], in_=sd.t[:]), r=[sd.b], w=[rstd.b])

    def epsc(self):
        return self.cst.t[:, CST['eps']:CST['eps'] + 1]

    def phaseA(self):
        nc, p, I = self.nc, self.p, self.I
        es = self.es
        self.xnT = self.sb(es, "xnT", [128, KC, T], BF16)
        with ExitStack() as les:
            NBA = 4
            xt = [self.sb(les, "xtA%d" % i, [128, D], F32) for i in range(NBA)]
            xs = [self.sb(les, "xsA%d" % i, [128, D], BF16) for i in range(NBA)]
            junk = self.sb(les, "junkA", [128, D], BF16)
            ssq = [self.sb(les, "ssqA%d" % i, [128, 1], F32) for i in range(NBA)]
            sd = [self.sb(les, "sdA%d" % i, [128, 1], F32) for i in range(NBA)]
            rstd = [self.sb(les, "rstdA%d" % i, [128, 1], F32) for i in range(NBA)]
            pT2 = [self.ps(les, "pTA%d" % i, [128, D], BF16) for i in range(2)]
            pT = [pT2[i % 2] for i in range(NBA)]
            gmix = self.pcol.t[:, PCOL['gmix']:PCOL['gmix'] + KC]
            for tt in range(NBA - 1):
                p.dma('sp', [(xt[tt].t[:], I['x'][tt * 128:(tt + 1) * 128, :])], w=[xt[tt].b])
            for tt in range(NT):
                b = tt % NBA
                t2 = tt + NBA - 1
                if t2 < NT:
                    p.dma('sp', [(xt[t2 % NBA].t[:], I['x'][t2 * 128:(t2 + 1) * 128, :])], w=[xt[t2 % NBA].b])
                self.rms_stats(xt[b].t[:], xt[b].b, junk, ssq[b], sd[b], rstd[b])
                p.op('pool', lambda e: e.tensor_scalar(out=xs[b].t[:], in0=xt[b].t[:], scalar1=rstd[b].t[:], scalar2=0.0,
                                                       op0=ALU.mult, op1=ALU.add),
                     r=[xt[b].b, rstd[b].b], w=[xs[b].b])
                for kc in range(KC):
                    p.op('pe', lambda e: e.transpose(pT[b].t[:, kc * 128:(kc + 1) * 128], xs[b].t[:, kc * 128:(kc + 1) * 128],
                                                     self.ident_bf.t[:]),
                         r=[xs[b].b, self.ident_bf.b], w=[pT[b].b], signal=(kc == KC - 1))
                p.op('dve', lambda e: e.tensor_tensor(
                    out=self.xnT.t[:, :, tt * 128:(tt + 1) * 128],
                    in0=pT[b].t[:].rearrange("p (k t) -> p k t", k=KC),
                    in1=gmix.unsqueeze(2).to_broadcast([128, KC, 128]), op=ALU.mult),
                     r=[pT[b].b, self.pcol.b], w=[self.xnT.b])
            p.barrier()
        self.dump('xnT', self.xnT, [128, KC, T], BF16)

    def phaseB_conv(self):
        nc, p, I = self.nc, self.p, self.I
        es = self.es
        self.u = self.sb(es, "u", [128, 8, T + 30], BF16)
        u = self.u
        xnT = self.xnT
        p.op('dve', lambda e: e.memset(u.t[:], 0.0), w=[u.b])
        with ExitStack() as les:
            conv = self.sb(les, "conv", [128, 8, T], F32)
            S1 = self.ps(les, "S1", [128, 512])
            S2 = self.ps(les, "S2", [128, 512])
            lw = ExitStack()
            diag1 = self.sb(lw, "diag", [128, CW, 128], BF16)
            diag = [diag1, diag1]
            wca = [self.sb(lw, "wca%d" % i, [128, KC, 128], BF16) for i in range(2)]
            wcb = [self.sb(lw, "wcb%d" % i, [128, KC, 128], BF16) for i in range(2)]
            sig = [self.sb(lw, "sig%d" % i, [128, 512], F32) for i in range(2)]
            psA = [self.ps(lw, "psA%d" % i, [128, 512]) for i in range(2)]
            psB = [self.ps(lw, "psB%d" % i, [128, 512]) for i in range(2)]
            psC = [self.ps(lw, "psC%d" % i, [128, 512]) for i in range(2)]
            idf = self.cst.t[:, CST['ident']:CST['ident'] + 128]
            for c in range(8):
                b = c % 2
                self.load_w(wca[b].t[:], wca[b].b, I['w_in'][:, OFF_CA + c * 128:OFF_CA + (c + 1) * 128])
                self.load_w(wcb[b].t[:], wcb[b].b, I['w_in'][:, OFF_CB + c * 128:OFF_CB + (c + 1) * 128])
                self.precast_next(after=psC[1].b)
                self.zero_fill_next(after=psC[1].b)
                dww = self.pcol.t[:, PCOL['dww'] + c * CW:PCOL['dww'] + (c + 1) * CW]
                p.op('dve', lambda e: e.tensor_tensor(out=diag[b].t[:], in0=idf.unsqueeze(1).to_broadcast([128, CW, 128]),
                                                      in1=dww.unsqueeze(2).to_broadcast([128, CW, 128]), op=ALU.mult),
                     r=[self.cst.b, self.pcol.b], w=[diag[b].b])
                for n in range(4):
                    nb = n % 2
                    ns = slice(n * 512, (n + 1) * 512)
                    for kc in range(KC):
                        p.op('pe', lambda e: e.matmul(psA[nb].t[:], wca[b].t[:, kc, :], xnT.t[:, kc, ns],
                                                      start=(kc == 0), stop=(kc == KC - 1)),
                             r=[wca[b].b, xnT.b], w=[psA[nb].b], signal=(kc == KC - 1))
                    for kc in range(KC):
                        p.op('pe', lambda e: e.matmul(psB[nb].t[:], wcb[b].t[:, kc, :], xnT.t[:, kc, ns],
                                                      start=(kc == 0), stop=(kc == KC - 1)),
                             r=[wcb[b].b, xnT.b], w=[psB[nb].b], signal=(kc == KC - 1))
                    p.op('act', lambda e: e.activation(out=sig[nb].t[:], in_=psB[nb].t[:], func=AF.Sigmoid),
                         r=[psB[nb].b], w=[sig[nb].b])
                    p.op('dve', lambda e: e.tensor_tensor(out=u.t[:, c, 30 + n * 512:30 + (n + 1) * 512], in0=psA[nb].t[:],
                                                          in1=sig[nb].t[:], op=ALU.mult),
                         r=[psA[nb].b, sig[nb].b], w=[u.b])
                for n in range(4):
                    nb = n % 2
                    for k in range(CW):
                        p.op('pe', lambda e: e.matmul(psC[nb].t[:], diag[b].t[:, k, :], u.t[:, c, n * 512 + k:n * 512 + k + 512],
                                                      start=(k == 0), stop=(k == CW - 1)),
                             r=[diag[b].b, u.b], w=[psC[nb].b], signal=(k == CW - 1))
                    p.op('act', lambda e: e.activation(out=conv.t[:, c, n * 512:(n + 1) * 512], in_=psC[nb].t[:],
                                                       func=AF.Identity, bias=self.pc('dwb', c)),
                         r=[psC[nb].b, self.pcol.b], w=[conv.b])
            p.barrier()
            lw.close()
            self.dump('u', u, [128, 8, T + 30], BF16)
            self.dump('conv', conv, [128, 8, T], F32)
            with ExitStack() as l2:
                sq = [self.sb(l2, "sq%d" % i, [128, 512], F32) for i in range(2)]
                mean = self.sb(l2, "mean", [128, 512], F32)
                msq = self.sb(l2, "msq", [128, 512], F32)
                var = self.sb(l2, "var", [128, 512], F32)
                rs = self.sb(l2, "rs", [128, 512], F32)
                tmp = [self.sb(l2, "tmpc%d" % i, [128, 512], F32) for i in range(2)]
                for n in range(4):
                    ns = slice(n * 512, (n + 1) * 512)
                    for c in range(8):
                        p.op('pe', lambda e: e.matmul(S1.t[:], self.ones_f.t[:], conv.t[:, c, ns], start=(c == 0), stop=(c == 7)),
                             r=[self.ones_f.b, conv.b], w=[S1.b], signal=(c == 7))
                    for c in range(8):
                        sb_ = c % 2
                        p.op('act', lambda e: e.activation(out=sq[sb_].t[:], in_=conv.t[:, c, ns], func=AF.Square),
                             r=[conv.b], w=[sq[sb_].b])
                        p.op('pe', lambda e: e.matmul(S2.t[:], self.ones_f.t[:], sq[sb_].t[:], start=(c == 0), stop=(c == 7)),
                             r=[self.ones_f.b, sq[sb_].b], w=[S2.b], signal=True)
                    p.op('act', lambda e: e.activation(out=msq.t[:], in_=S1.t[:], func=AF.Square, scale=1.0 / CONV_CH),
                         r=[S1.b], w=[msq.b])
                    p.op('act', lambda e: e.activation(out=mean.t[:], in_=S1.t[:], func=AF.Copy, scale=1.0 / CONV_CH),
                         r=[S1.b], w=[mean.b])
                    p.op('dve', lambda e: e.scalar_tensor_tensor(out=var.t[:], in0=S2.t[:], scalar=1.0 / CONV_CH, in1=msq.t[:],
                                                                 op0=ALU.mult, op1=ALU.subtract),
                         r=[S2.b, msq.b], w=[var.b])
                    p.op('act', lambda e: e.activation(out=var.t[:], in_=var.t[:], func=AF.Sqrt, bias=self.epsc(), scale=1.0),
                         r=[var.b, self.cst.b], w=[var.b])
                    p.op('dve', lambda e: e.reciprocal(out=rs.t[:], in_=var.t[:]), r=[var.b], w=[rs.b])
                    for c in range(8):
                        tb_ = c % 2
                        p.op('dve', lambda e: e.tensor_tensor(out=tmp[tb_].t[:], in0=conv.t[:, c, ns], in1=mean.t[:], op=ALU.subtract),
                             r=[conv.b, mean.b], w=[tmp[tb_].b])
                        p.op('dve', lambda e: e.tensor_tensor(out=tmp[tb_].t[:], in0=tmp[tb_].t[:], in1=rs.t[:], op=ALU.mult),
                             r=[tmp[tb_].b, rs.b], w=[tmp[tb_].b])
                        p.op('act', lambda e: e.activation(out=u.t[:, c, 30 + n * 512:30 + (n + 1) * 512], in_=tmp[tb_].t[:],
                                                           func=AF.Silu, bias=self.pc('lnb', c), scale=self.pc('lng', c)),
                             r=[tmp[tb_].b, self.pcol.b], w=[u.b])
            p.barrier()
        self.dump('aT', u, [128, 8, T + 30], BF16)

    def qk_chunk(self, w, dst_ap, dstb, gcol, gcolb, psQ, psS, sqb, rr):
        for n in range(4):
            self.qk_part(w, dst_ap, dstb, gcol, gcolb, psQ, psS, sqb, rr, n)

    def qk_part(self, w, dst_ap, dstb, gcol, gcolb, psQ, psS, sqb, rr, n):
        p = self.p
        xnT = self.xnT
        if True:
            nb = n % 2
            ns = slice(n * 512, (n + 1) * 512)
            for kc in range(KC):
                p.op('pe', lambda e: e.matmul(psQ[nb].t[:], w.t[:, kc, :], xnT.t[:, kc, ns], start=(kc == 0), stop=(kc == KC - 1)),
                     r=[w.b, xnT.b], w=[psQ[nb].b], signal=(kc == KC - 1))
            p.op('act', lambda e: e.activation(out=sqb[nb].t[:], in_=psQ[nb].t[:], func=AF.Square), r=[psQ[nb].b], w=[sqb[nb].b])
            p.op('pe', lambda e: e.matmul(psS.t[:], self.blk_bf.t[:], sqb[nb].t[:], start=True, stop=True),
                 r=[self.blk_bf.b, sqb[nb].b], w=[psS.b])
            p.op('act', lambda e: e.activation(out=rr[nb].t[:], in_=psS.t[:], func=AF.Ln, bias=self.epsc(), scale=1.0 / HD),
                 r=[psS.b, self.cst.b], w=[rr[nb].b])
            p.op('act', lambda e: e.activation(out=rr[nb].t[:], in_=rr[nb].t[:], func=AF.Exp, scale=-0.5), r=[rr[nb].b], w=[rr[nb].b])
            p.op('dve', lambda e: e.scalar_tensor_tensor(out=dst_ap[:, ns], in0=psQ[nb].t[:], scalar=gcol, in1=rr[nb].t[:],
                                                         op0=ALU.mult, op1=ALU.mult),
                 r=[psQ[nb].b, rr[nb].b, gcolb], w=[dstb])

    def phaseC(self):
        nc, p, I = self.nc, self.p, self.I
        xnT, aT, oT = self.xnT, self.u, self.oT
        self.mergedS = nc.dram_tensor("mergedS", [128, KC, T], BF16, kind="Internal").ap()
        self.mergedSb = p.buf("mergedS")
        with ExitStack() as les:
            wco = [self.sb(les, "wco%d" % i, [128, 8, 128], BF16) for i in range(2)]
            wao = [self.sb(les, "wao%d" % i, [128, 8, 128], BF16) for i in range(2)]
            wg0 = [self.sb(les, "wg0%d" % i, [128, KC, 128], BF16) for i in range(2)]
            wg1 = [self.sb(les, "wg1%d" % i, [128, KC, 128], BF16) for i in range(2)]
            s0 = [self.sb(les, "s0%d" % i, [128, 512], F32) for i in range(2)]
            s1 = [self.sb(les, "s1%d" % i, [128, 512], F32) for i in range(2)]
            mg = [self.sb(les, "mg%d" % i, [128, T], BF16) for i in range(2)]
            psYc = [self.ps(les, "psYc%d" % i, [128, 512]) for i in range(2)]
            psYa = [self.ps(les, "psYa%d" % i, [128, 512]) for i in range(2)]
            psG0 = [self.ps(les, "psG0%d" % i, [128, 512]) for i in range(2)]
            psG1 = [self.ps(les, "psG1%d" % i, [128, 512]) for i in range(2)]
            def loadC(j):
                b = j % 2
                cs = slice(j * 128, (j + 1) * 128)
                self.load_w(wg0[b].t[:], wg0[b].b, I['w_in'][:, OFF_G + j * 128:OFF_G + (j + 1) * 128])
                self.load_w(wg1[b].t[:], wg1[b].b, I['w_in'][:, OFF_G + D + j * 128:OFF_G + D + (j + 1) * 128])
                self.load_w(wco[b].t[:], wco[b].b, I['w_conv_out'][:, cs])
                self.load_w(wao[b].t[:], wao[b].b, I['w_attn_out'][:, cs])
                self.precast_next(after=psYa[1].b)
            self.zero_fill_next(100)
            loadC(0)
            for j in range(KC):
                b = j % 2
                if j + 1 < KC:
                    loadC(j + 1)
                for n in range(4):
                    nb = n % 2
                    ns = slice(n * 512, (n + 1) * 512)
                    for kc in range(KC):
                        p.op('pe', lambda e: e.matmul(psG0[nb].t[:], wg0[b].t[:, kc, :], xnT.t[:, kc, ns], start=(kc == 0), stop=(kc == KC - 1)),
                             r=[wg0[b].b, xnT.b], w=[psG0[nb].b], signal=(kc == KC - 1))
                    for kc in range(KC):
                        p.op('pe', lambda e: e.matmul(psG1[nb].t[:], wg1[b].t[:, kc, :], xnT.t[:, kc, ns], start=(kc == 0), stop=(kc == KC - 1)),
                             r=[wg1[b].b, xnT.b], w=[psG1[nb].b], signal=(kc == KC - 1))
                    for kc in range(8):
                        p.op('pe', lambda e: e.matmul(psYc[nb].t[:], wco[b].t[:, kc, :], aT.t[:, kc, 30 + n * 512:30 + (n + 1) * 512],
                                                      start=(kc == 0), stop=(kc == 7)),
                             r=[wco[b].b, aT.b], w=[psYc[nb].b], signal=(kc == 7))
                    for kc in range(8):
                        p.op('pe', lambda e: e.matmul(psYa[nb].t[:], wao[b].t[:, kc, :], oT.t[:, kc, ns], start=(kc == 0), stop=(kc == 7)),
                             r=[wao[b].b, oT.b], w=[psYa[nb].b], signal=(kc == 7))
                    p.op('act', lambda e: e.activation(out=s0[nb].t[:], in_=psG0[nb].t[:], func=AF.Sigmoid), r=[psG0[nb].b], w=[s0[nb].b])
                    p.op('act', lambda e: e.activation(out=s1[nb].t[:], in_=psG1[nb].t[:], func=AF.Sigmoid), r=[psG1[nb].b], w=[s1[nb].b])
                    p.op('dve', lambda e: e.tensor_tensor(out=s0[nb].t[:], in0=psYc[nb].t[:], in1=s0[nb].t[:], op=ALU.mult),
                         r=[psYc[nb].b, s0[nb].b], w=[s0[nb].b])
                    p.op('dve', lambda e: e.tensor_tensor(out=s1[nb].t[:], in0=psYa[nb].t[:], in1=s1[nb].t[:], op=ALU.mult),
                         r=[psYa[nb].b, s1[nb].b], w=[s1[nb].b])
                    p.op('pool', lambda e: e.tensor_tensor(out=mg[b].t[:, ns], in0=s0[nb].t[:], in1=s1[nb].t[:], op=ALU.add),
                         r=[s0[nb].b, s1[nb].b], w=[mg[b].b])
                p.dma('sp', [(self.mergedS[:, j, :], mg[b].t[:])], r=[mg[b].b], w=[self.mergedSb])
            p.barrier()
        if 'merged' in self.debug:
            o = nc.dram_tensor("dbg_merged", [128, KC, T], BF16, kind="ExternalOutput").ap()
            ob = p.buf("dbg_merged")
            p.dma('sp', [(o, self.mergedS)], r=[self.mergedSb], w=[ob])
            self.dbg_out['merged'] = ob

    def phaseM(self):
        nc, p, I = self.nc, self.p, self.I
        kxT, vx = self.kxT, self.vx
        with ExitStack() as l2:
            memnT = self.sb(l2, "memnT", [128, KC, MEM], BF16)
            mt = self.sb(l2, "mt", [128, D], F32)
            ms = self.sb(l2, "ms", [128, D], BF16)
            junk = self.sb(l2, "junkM", [128, D], BF16)
            ssq = self.sb(l2, "ssqM", [128, 1], F32)
            sd = self.sb(l2, "sdM", [128, 1], F32)
            rstd = self.sb(l2, "rstdM", [128, 1], F32)
            wkv = self.sb(l2, "wkv", [128, KC, XW], BF16)
            sqk = self.sb(l2, "sqk", [128, MEM], BF16)
            rk = self.sb(l2, "rk", [128, MEM], F32)
            xkgs = self.sb(l2, "xkgs", [128, 1], F32)
            pTm = self.ps(l2, "pTm", [128, D], BF16)
            psK = self.ps(l2, "psK", [128, 512])
            psK2 = self.ps(l2, "psK2", [128, 512])
            gmem = self.pcol.t[:, PCOL['gmem']:PCOL['gmem'] + KC]
            for mtile in range(2):
                p.dma('sp', [(mt.t[:], I['mem'][mtile * 128:(mtile + 1) * 128, :])], w=[mt.b])
                self.rms_stats(mt.t[:], mt.b, junk, ssq, sd, rstd)
                p.op('act', lambda e: e.activation(out=ms.t[:], in_=mt.t[:], func=AF.Copy, scale=rstd.t[:]),
                     r=[mt.b, rstd.b], w=[ms.b])
                for kc in range(KC):
                    p.op('pe', lambda e: e.transpose(pTm.t[:, kc * 128:(kc + 1) * 128], ms.t[:, kc * 128:(kc + 1) * 128], self.ident_bf.t[:]),
                         r=[ms.b, self.ident_bf.b], w=[pTm.b], signal=(kc == KC - 1))
                p.op('dve', lambda e: e.tensor_tensor(out=memnT.t[:, :, mtile * 128:(mtile + 1) * 128],
                                                      in0=pTm.t[:].rearrange("p (k t) -> p k t", k=KC),
                                                      in1=gmem.unsqueeze(2).to_broadcast([128, KC, 128]), op=ALU.mult),
                     r=[pTm.b, self.pcol.b], w=[memnT.b])
            p.op('dve', lambda e: e.tensor_scalar(out=xkgs.t[:], in0=self.pc('xkg'), scalar1=XHD ** -0.5, scalar2=None, op0=ALU.mult),
                 r=[self.pcol.b], w=[xkgs.b])
            self.load_w(wkv.t[:], wkv.b, I['w_xkv'][:, 0:XW])
            for hh in range(XH):
                for kc in range(KC):
                    p.op('pe', lambda e: e.matmul(psK.t[:, 0:MEM], wkv.t[:, kc, hh * 128:(hh + 1) * 128], memnT.t[:, kc, :],
                                                  start=(kc == 0), stop=(kc == KC - 1)),
                         r=[wkv.b, memnT.b], w=[psK.b], signal=(kc == KC - 1))
                p.op('act', lambda e: e.activation(out=sqk.t[:], in_=psK.t[:, 0:MEM], func=AF.Square), r=[psK.b], w=[sqk.b])
                p.op('pe', lambda e: e.matmul(psK2.t[:, 0:MEM], self.ones_bf.t[:], sqk.t[:], start=True, stop=True),
                     r=[self.ones_bf.b, sqk.b], w=[psK2.b])
                p.op('act', lambda e: e.activation(out=rk.t[:], in_=psK2.t[:, 0:MEM], func=AF.Sqrt, bias=self.epsc(), scale=1.0 / XHD),
                     r=[psK2.b, self.cst.b], w=[rk.b])
                p.op('dve', lambda e: e.reciprocal(out=rk.t[:], in_=rk.t[:]), r=[rk.b], w=[rk.b])
                p.op('dve', lambda e: e.scalar_tensor_tensor(out=kxT.t[:, hh, :], in0=psK.t[:, 0:MEM], scalar=xkgs.t[:], in1=rk.t[:],
                                                             op0=ALU.mult, op1=ALU.mult),
                     r=[psK.b, xkgs.b, rk.b], w=[kxT.b])
            self.load_w(wkv.t[:], wkv.b, I['w_xkv'][:, XW:2 * XW])
            for mc in range(2):
                for kc in range(KC):
                    p.op('pe', lambda e: e.matmul(psK.t[:], memnT.t[:, kc, mc * 128:(mc + 1) * 128], wkv.t[:, kc, :],
                                                  start=(kc == 0), stop=(kc == KC - 1)),
                         r=[wkv.b, memnT.b], w=[psK.b], signal=(kc == KC - 1))
                p.op('act', lambda e: e.activation(out=vx.t[:, mc, :], in_=psK.t[:], func=AF.Copy), r=[psK.b], w=[vx.b])
            p.barrier()

    def phaseD(self):
        nc, p, I = self.nc, self.p, self.I
        es = self.es
        self.h2S = nc.dram_tensor("h2S", [T, D], F32, kind="Internal").ap()
        self.h2Sb = p.buf("h2S")
        self.logits = self.sb(es, "logits", [128, NT, 36], F32)
        self.rstd2 = self.sb(es, "rstd2", [128, NT], F32)
        logits, rstd2 = self.logits, self.rstd2
        with ExitStack() as les:
            kxT, vx = self.kxT, self.vx
            wmix = self.sb(les, "wmix", [128, KC, D], BF16)
            wxq = self.sb(les, "wxq", [128, KC, XW], BF16)
            wxo = self.sb(les, "wxo", [128, XH, D], BF16)
            wrt = self.sb(les, "wrt", [128, KC, 36], F32)
            wmixb = [p.buf("wmix_cg%d" % cg) for cg in range(4)]
            for cg in range(4):
                p.dma('pool', [(wmix.t[:, :, cg * 512:(cg + 1) * 512],
                                I['w_mix_out'][:, cg * 512:(cg + 1) * 512].rearrange("(k p) c -> p k c", p=128))], w=[wmixb[cg]])
            self.load_w(wxq.t[:], wxq.b, I['w_xq'])
            for cg in range(4):
                p.dma('pool', [(wxo.t[:, :, cg * 512:(cg + 1) * 512],
                                I['w_xo'][:, cg * 512:(cg + 1) * 512].rearrange("(k p) c -> p k c", p=128))], w=[wxo.b])
            p.dma('sp', [(wrt.t[:], I['w_rt'].rearrange("(k p) c -> p k c", p=128))], w=[wrt.b])
            gx = self.pcol.t[:, PCOL['gx']:PCOL['gx'] + KC]
            gmoe = self.pcol.t[:, PCOL['gmoe']:PCOL['gmoe'] + KC]
            xqg = self.prow.t[:, PROW['xqg']:PROW['xqg'] + XW]
            rtb = self.prow.t[:, PROW['rtb']:PROW['rtb'] + 36]
            idf = self.cst.t[:, CST['ident']:CST['ident'] + 128]

            class Lane:
                pass
            lanes = []
            for li in range(2):
                L = Lane()
                n = lambda s_: "%s_%d" % (s_, li)
                L.mgt = self.sb(les, n("mgt"), [128, KC, 128], BF16)
                L.xt = self.sb(les, n("xtD"), [128, D], F32)
                L.h = self.sb(les, n("hD"), [128, D], F32)
                L.xs = self.sb(les, n("xsD"), [128, D], BF16)
                L.hn1T = self.sb(les, n("hn1T"), [128, KC, 128], BF16)
                L.hn2T = self.sb(les, n("hn2T"), [128, KC, 128], F32)
                L.ssq = self.sb(les, n("ssqD"), [128, 1], F32)
                L.sd = self.sb(les, n("sdD"), [128, 1], F32)
                L.rstd = self.sb(les, n("rstdD"), [128, 1], F32)
                L.sq = self.sb(les, n("sqD"), [128, XW], F32)
                L.ssq4 = self.sb(les, n("ssq4"), [128, XH], F32)
                L.r4 = self.sb(les, n("r4"), [128, XH], F32)
                L.qf = self.sb(les, n("qf"), [128, XW], F32)
                L.qn = self.sb(les, n("qn"), [128, XW], BF16)
                L.qxT = self.sb(les, n("qxT"), [128, XH, 128], BF16)
                L.pxT = self.sb(les, n("pxT"), [128, XH, 2, 128], BF16)
                L.rdx = self.sb(les, n("rdx"), [128, XW], F32)
                L.oxT = self.sb(les, n("oxT"), [128, XH, 128], BF16)
                L.PA = self.ps(les, n("PA"), [128, D])
                L.B01 = L.PA.b
                L.B2 = p.buf(n("PA_b2"))
                L.B3 = p.buf(n("PA_b3"))
                L.PAll = [L.B01, L.B2, L.B3]
                L.P01 = L.PA.t[:, 0:1024]
                L.P01b = L.PA.t[:, 0:1024].bitcast(BF16)
                L.P2 = L.PA.t[:, 1024:1536]
                L.P3 = L.PA.t[:, 1536:2048]
                L.P3b = L.PA.t[:, 1536:2048].bitcast(BF16)
                lanes.append(L)

            def s_load(L, tt):
                ts_ = slice(tt * 128, (tt + 1) * 128)
                p.dma('sp', [(L.mgt.t[:], self.mergedS[:, :, ts_])], r=[self.mergedSb], w=[L.mgt.b])
                p.dma('sp', [(L.xt.t[:], I['x'][ts_, :])], w=[L.xt.b])

            def s_mix(L, tt):
                for cg in range(4):
                    for kc in range(KC):
                        p.op('pe', lambda e: e.matmul(L.PA.t[:, cg * 512:(cg + 1) * 512], L.mgt.t[:, kc, :], wmix.t[:, kc, cg * 512:(cg + 1) * 512],
                                                      start=(kc == 0), stop=(kc == KC - 1)),
                             r=[L.mgt.b, wmixb[cg]], w=L.PAll, signal=(cg == 3 and kc == KC - 1))
                p.op('dve', lambda e: e.tensor_tensor(out=L.h.t[:], in0=L.PA.t[:], in1=L.xt.t[:], op=ALU.add), r=L.PAll + [L.xt.b], w=[L.h.b])

            def stats(L):
                p.op('act', lambda e: e.activation(out=L.xs.t[:], in_=L.h.t[:], func=AF.Square, accum_out=L.ssq.t[:]),
                     r=[L.h.b], w=[L.xs.b, L.ssq.b])
                p.op('act', lambda e: e.activation(out=L.sd.t[:], in_=L.ssq.t[:], func=AF.Sqrt, bias=self.epsc(), scale=1.0 / D),
                     r=[L.ssq.b, self.cst.b], w=[L.sd.b])
                p.op('dve', lambda e: e.reciprocal(out=L.rstd.t[:], in_=L.sd.t[:]), r=[L.sd.b], w=[L.rstd.b])

            def s_norm1(L, tt):
                stats(L)
                p.op('act', lambda e: e.activation(out=L.xs.t[:], in_=L.h.t[:], func=AF.Copy, scale=L.rstd.t[:]), r=[L.h.b, L.rstd.b], w=[L.xs.b])

            def s_tr1(L, tt):
                for kc in range(KC):
                    p.op('pe', lambda e: e.transpose(L.P01b[:, kc * 128:(kc + 1) * 128], L.xs.t[:, kc * 128:(kc + 1) * 128], self.ident_bf.t[:]),
                         r=[L.xs.b, self.ident_bf.b], w=[L.B01], signal=(kc == KC - 1))
                p.op('dve', lambda e: e.tensor_tensor(out=L.hn1T.t[:], in0=L.P01b.rearrange("p (k t) -> p k t", k=KC),
                                                      in1=gx.unsqueeze(2).to_broadcast([128, KC, 128]), op=ALU.mult),
                     r=[L.B01, self.pcol.b], w=[L.hn1T.b])

            def s_qa(L, tt):
                for kc in range(KC):
                    p.op('pe', lambda e: e.matmul(L.P2, L.hn1T.t[:, kc, :], wxq.t[:, kc, :], start=(kc == 0), stop=(kc == KC - 1)),
                         r=[L.hn1T.b, wxq.b], w=[L.B2], signal=(kc == KC - 1))
                p.op('act', lambda e: e.activation(out=L.sq.t[:], in_=L.P2, func=AF.Square), r=[L.B2], w=[L.sq.b])
                p.op('dve', lambda e: e.tensor_reduce(out=L.ssq4.t[:], in_=L.sq.t[:].rearrange("p (h d) -> p h d", h=XH), axis=AX.X, op=ALU.add),
                     r=[L.sq.b], w=[L.ssq4.b])
                p.op('act', lambda e: e.activation(out=L.r4.t[:], in_=L.ssq4.t[:], func=AF.Sqrt, bias=self.epsc(), scale=1.0 / XHD),
                     r=[L.ssq4.b, self.cst.b], w=[L.r4.b])
                p.op('dve', lambda e: e.reciprocal(out=L.r4.t[:], in_=L.r4.t[:]), r=[L.r4.b], w=[L.r4.b])
                p.op('dve', lambda e: e.tensor_tensor(out=L.qf.t[:].rearrange("p (h d) -> p h d", h=XH),
                                                      in0=L.P2.rearrange("p (h d) -> p h d", h=XH),
                                                      in1=L.r4.t[:].unsqueeze(2).to_broadcast([128, XH, XHD]), op=ALU.mult),
                     r=[L.B2, L.r4.b], w=[L.qf.b])
                p.op('dve', lambda e: e.tensor_tensor(out=L.qn.t[:], in0=L.qf.t[:], in1=xqg, op=ALU.mult), r=[L.qf.b, self.prow.b], w=[L.qn.b])

            def s_qb(L, tt):
                for hh in range(XH):
                    p.op('pe', lambda e: e.transpose(L.P3b[:, hh * 128:(hh + 1) * 128], L.qn.t[:, hh * 128:(hh + 1) * 128], self.ident_bf.t[:]),
                         r=[L.qn.b, self.ident_bf.b], w=[L.B3], signal=(hh == XH - 1))
                p.op('act', lambda e: e.activation(out=L.qxT.t[:], in_=L.P3b[:, 0:XW].rearrange("p (h t) -> p h t", h=XH), func=AF.Copy),
                     r=[L.B3], w=[L.qxT.b])

            def s_at_a(L, tt):
                for hh in range(XH):
                    for mc in range(2):
                        o = (hh * 2 + mc) * 128
                        p.op('pe', lambda e: e.matmul(L.P01[:, o:o + 128], kxT.t[:, hh, mc * 128:(mc + 1) * 128], L.qxT.t[:, hh, :],
                                                      start=True, stop=True),
                             r=[kxT.b, L.qxT.b], w=[L.B01], signal=(hh == XH - 1 and mc == 1))
                p.op('act', lambda e: e.activation(out=L.pxT.t[:].rearrange("p h m t -> p (h m t)"), in_=L.P01, func=AF.Exp),
                     r=[L.B01], w=[L.pxT.b])

            def s_at_b(L, tt):
                for (pso, psb, get_l) in ((L.P2, L.B2, lambda hh, mc: vx.t[:, mc, hh * 128:(hh + 1) * 128]),
                                          (L.P3, L.B3, lambda hh, mc: self.ones_bf.t[:])):
                    for hh in range(XH):
                        for mc in range(2):
                            p.op('pe', lambda e: e.matmul(pso[:, hh * 128:(hh + 1) * 128], get_l(hh, mc), L.pxT.t[:, hh, mc, :],
                                                          start=(mc == 0), stop=(mc == 1)),
                                 r=[vx.b, self.ones_bf.b, L.pxT.b], w=[psb], signal=(hh == XH - 1 and mc == 1))
                p.op('dve', lambda e: e.reciprocal(out=L.rdx.t[:], in_=L.P3), r=[L.B3], w=[L.rdx.b])
                p.op('dve', lambda e: e.tensor_tensor(out=L.oxT.t[:].rearrange("p h t -> p (h t)"), in0=L.P2, in1=L.rdx.t[:], op=ALU.mult),
                     r=[L.B2, L.rdx.b], w=[L.oxT.b])

            def s_xo(L, tt):
                ts_ = slice(tt * 128, (tt + 1) * 128)
                for cg in range(4):
                    for hh in range(XH):
                        p.op('pe', lambda e: e.matmul(L.PA.t[:, cg * 512:(cg + 1) * 512], L.oxT.t[:, hh, :], wxo.t[:, hh, cg * 512:(cg + 1) * 512],
                                                      start=(hh == 0), stop=(hh == XH - 1)),
                             r=[L.oxT.b, wxo.b], w=L.PAll, signal=(cg == 3 and hh == XH - 1))
                p.op('dve', lambda e: e.tensor_tensor(out=L.h.t[:], in0=L.PA.t[:], in1=L.h.t[:], op=ALU.add), r=L.PAll + [L.h.b], w=[L.h.b])
                p.dma('sp', [(self.h2S[ts_, :], L.h.t[:])], r=[L.h.b], w=[self.h2Sb])
                stats(L)
                p.op('dve', lambda e: e.tensor_copy(out=rstd2.t[:, tt:tt + 1], in_=L.rstd.t[:]), r=[L.rstd.b], w=[rstd2.b])
                p.op('act', lambda e: e.activation(out=L.h.t[:], in_=L.h.t[:], func=AF.Copy, scale=L.rstd.t[:]), r=[L.h.b, L.rstd.b], w=[L.h.b])

            def s_tr2(L, tt):
                for kc in range(KC):
                    p.op('pe', lambda e: e.transpose(L.PA.t[:, kc * 128:(kc + 1) * 128], L.h.t[:, kc * 128:(kc + 1) * 128], idf),
                         r=[L.h.b, self.cst.b], w=L.PAll, signal=(kc == KC - 1))
                p.op('dve', lambda e: e.tensor_tensor(out=L.hn2T.t[:], in0=L.PA.t[:].rearrange("p (k t) -> p k t", k=KC),
                                                      in1=gmoe.unsqueeze(2).to_broadcast([128, KC, 128]), op=ALU.mult),
                     r=L.PAll + [self.pcol.b], w=[L.hn2T.b])

            def s_rt(L, tt):
                for kc in range(KC):
                    p.op('pe', lambda e: e.matmul(L.P2[:, 0:36], L.hn2T.t[:, kc, :], wrt.t[:, kc, :], start=(kc == 0), stop=(kc == KC - 1)),
                         r=[L.hn2T.b, wrt.b], w=[L.B2], signal=(kc == KC - 1))
                p.op('dve', lambda e: e.tensor_tensor(out=logits.t[:, tt, :], in0=L.P2[:, 0:36], in1=rtb, op=ALU.add),
                     r=[L.B2, self.prow.b], w=[logits.b])

            stages = [s_mix, s_norm1, s_tr1, s_qa, s_qb, s_at_a, s_at_b, s_xo, s_tr2, s_rt]
            s_load(lanes[0], 0)
            s_load(lanes[1], 1)
            for pi in range(NT // 2):
                t0, t1 = 2 * pi, 2 * pi + 1
                for st in stages:
                    st(lanes[0], t0)
                    st(lanes[1], t1)
                    if st is s_mix:
                        if pi + 1 < NT // 2:
                            s_load(lanes[0], t0 + 2)
                            s_load(lanes[1], t1 + 2)
            p.barrier()
        self.dump('logits', logits, [128, NT, 36], F32)
        if 'h2' in self.debug:
            o = nc.dram_tensor("dbg_h2", [T, D], F32, kind="ExternalOutput").ap()
            ob = p.buf("dbg_h2")
            p.dma('sp', [(o, self.h2S)], r=[self.h2Sb], w=[ob])
            self.dbg_out['h2'] = ob

    def phaseR(self):
        nc, p, I = self.nc, self.p, self.I
        es = self.es
        self.precast_next(NE)
        L = self.logits
        NR = NE * CAP
        self.ysS = nc.dram_tensor("ysS", [NR + 1, D], BF16, kind="Internal").ap()
        self.xs_t = [p.buf("xs_t%d" % i) for i in range(4)]
        self.ys_zero = p.buf("ys_zero")
        self.gate = [self.sb(es, "gate%d" % k, [128, NT], F32) for k in range(2)]
        self.dsti = [self.sb(es, "dsti%d" % k, [128, NT], I32) for k in range(2)]
        self.gmoe_row = self.sb(es, "gmoe_row", [128, D], F32)
        p.dma('sp', [(self.gmoe_row.t[:], I['gmoe_row'])], w=[self.gmoe_row.b])
        with ExitStack() as les:
            zt = self.sb(les, "zt", [128, D], F32)
            p.op('dve', lambda e: e.memset(zt.t[:], 0.0), w=[zt.b])
            ztb = zt.t[:].bitcast(BF16)
            p.dma('sp', [(self.ysS[NR:NR + 1, :], ztb[0:1, 0:D])], r=[zt.b], w=[self.ys_zero])

            def t2(name, n, dt=F32):
                return self.sb(les, name, [128, NT, n] if n else [128, NT], dt)
            lg = L.t[:, :, 0:NG]
            le4 = L.t[:, :, NG:NG + NE].rearrange("p t (g e) -> p t g e", g=NG)
            gmax = t2("gmax", 0)
            ohg = t2("ohg", NG)
            lgs = t2("lgs", NG)
            sum4 = t2("sum4", 0)
            pg = t2("pg", 0)
            tmp48 = t2("tmp48", NE)
            lesel = t2("lesel", EPG)
            lesel2 = t2("lesel2", EPG)
            m1 = t2("m1", 0)
            m2 = t2("m2", 0)
            oh = [t2("oh1", EPG), t2("oh2", EPG)]
            dm = t2("dm", 0)
            w1 = t2("w1", 0)
            Mk = [t2("Mk1", NE), t2("Mk2", NE)]
            Mb = t2("Mb", NE, BF16)
            tot = t2("tot", NE)
            carry = t2("carry", NE)
            pos = t2("pos", NE)
            pose = t2("pose", NE)
            ecap = self.sb(les, "ecap", [128, NE], F32)
            posk = t2("posk", 0)
            dk = t2("dk", 0)
            valid = t2("valid", 0)
            tri_bf = self.sb(les, "tri_bf", [128, 128], BF16)
            psP = self.ps(les, "psP", [128, 512])
            psTot = self.ps(les, "psTot", [128, 512])
            V = 'dve'

            def bc(ap2, n):
                return ap2.unsqueeze(2).to_broadcast([128, NT, n])
            p.op(V, lambda e: e.tensor_copy(out=tri_bf.t[:], in_=self.cst.t[:, CST['tri']:CST['tri'] + 128]), r=[self.cst.b], w=[tri_bf.b])
            p.op(V, lambda e: e.tensor_reduce(out=gmax.t[:], in_=lg, axis=AX.X, op=ALU.max), r=[L.b], w=[gmax.b])
            p.op(V, lambda e: e.tensor_tensor(out=ohg.t[:], in0=lg, in1=bc(gmax.t[:], NG), op=ALU.is_equal), r=[L.b, gmax.b], w=[ohg.b])
            p.op(V, lambda e: e.tensor_tensor(out=lgs.t[:], in0=lg, in1=bc(gmax.t[:], NG), op=ALU.subtract), r=[L.b, gmax.b], w=[lgs.b])
            p.op('act', lambda e: e.activation(out=lgs.t[:], in_=lgs.t[:], func=AF.Exp), r=[lgs.b], w=[lgs.b])
            p.op(V, lambda e: e.tensor_reduce(out=sum4.t[:], in_=lgs.t[:], axis=AX.X, op=ALU.add), r=[lgs.b], w=[sum4.b])
            p.op(V, lambda e: e.reciprocal(out=pg.t[:], in_=sum4.t[:]), r=[sum4.b], w=[pg.b])
            t48 = tmp48.t[:].rearrange("p t (g e) -> p t g e", g=NG)
            p.op(V, lambda e: e.tensor_tensor(out=t48, in0=le4, in1=ohg.t[:].unsqueeze(3).to_broadcast([128, NT, NG, EPG]), op=ALU.mult),
                 r=[L.b, ohg.b], w=[tmp48.b])
            p.op(V, lambda e: e.tensor_reduce(out=lesel.t[:], in_=tmp48.t[:].rearrange("p t (g e) -> p t e g", g=NG), axis=AX.X, op=ALU.add),
                 r=[tmp48.b], w=[lesel.b])
            p.op(V, lambda e: e.tensor_reduce(out=m1.t[:], in_=lesel.t[:], axis=AX.X, op=ALU.max), r=[lesel.b], w=[m1.b])
            p.op(V, lambda e: e.tensor_tensor(out=oh[0].t[:], in0=lesel.t[:], in1=bc(m1.t[:], EPG), op=ALU.is_equal),
                 r=[lesel.b, m1.b], w=[oh[0].b])
            p.op(V, lambda e: e.scalar_tensor_tensor(out=lesel2.t[:], in0=oh[0].t[:], scalar=-1e30, in1=lesel.t[:], op0=ALU.mult, op1=ALU.add),
                 r=[oh[0].b, lesel.b], w=[lesel2.b])
            p.op(V, lambda e: e.tensor_reduce(out=m2.t[:], in_=lesel2.t[:], axis=AX.X, op=ALU.max), r=[lesel2.b], w=[m2.b])
            p.op(V, lambda e: e.tensor_tensor(out=oh[1].t[:], in0=lesel2.t[:], in1=bc(m2.t[:], EPG), op=ALU.is_equal),
                 r=[lesel2.b, m2.b], w=[oh[1].b])
            p.op(V, lambda e: e.tensor_tensor(out=dm.t[:], in0=m2.t[:], in1=m1.t[:], op=ALU.subtract), r=[m1.b, m2.b], w=[dm.b])
            p.op('act', lambda e: e.activation(out=dm.t[:], in_=dm.t[:], func=AF.Exp), r=[dm.b], w=[dm.b])
            p.op(V, lambda e: e.tensor_scalar(out=w1.t[:], in0=dm.t[:], scalar1=1.0, scalar2=None, op0=ALU.add), r=[dm.b], w=[w1.b])
            p.op(V, lambda e: e.reciprocal(out=w1.t[:], in_=w1.t[:]), r=[w1.b], w=[w1.b])
            g1, g2 = self.gate
            p.op(V, lambda e: e.tensor_tensor(out=g1.t[:], in0=pg.t[:], in1=w1.t[:], op=ALU.mult), r=[pg.b, w1.b], w=[g1.b])
            p.op(V, lambda e: e.tensor_tensor(out=g2.t[:], in0=g1.t[:], in1=dm.t[:], op=ALU.mult), r=[g1.b, dm.b], w=[g2.b])
            for k in range(2):
                mk4 = Mk[k].t[:].rearrange("p t (g e) -> p t g e", g=NG)
                p.op(V, lambda e: e.tensor_tensor(out=mk4, in0=ohg.t[:].unsqueeze(3).to_broadcast([128, NT, NG, EPG]),
                                                  in1=oh[k].t[:].unsqueeze(2).to_broadcast([128, NT, NG, EPG]), op=ALU.mult),
                     r=[ohg.b, oh[k].b], w=[Mk[k].b])
            p.op(V, lambda e: e.tensor_tensor(out=Mb.t[:], in0=Mk[0].t[:], in1=Mk[1].t[:], op=ALU.add), r=[Mk[0].b, Mk[1].b], w=[Mb.b])
            Mbf = Mb.t[:].rearrange("p t e -> p (t e)")
            p.op('pe', lambda e: e.matmul(psP.t[:], tri_bf.t[:], Mbf, start=True, stop=True), r=[tri_bf.b, Mb.b], w=[psP.b])
            p.op('pe', lambda e: e.matmul(psTot.t[:], self.ones_bf.t[:], Mbf, start=True, stop=True), r=[self.ones_bf.b, Mb.b], w=[psTot.b])
            p.op(V, lambda e: e.tensor_copy(out=tot.t[:].rearrange("p t e -> p (t e)"), in_=psTot.t[:]), r=[psTot.b], w=[tot.b])
            p.op(V, lambda e: e.memset(carry.t[:, 0, :], 0.0), w=[carry.b])
            for tt in range(1, NT):
                p.op(V, lambda e: e.tensor_tensor(out=carry.t[:, tt, :], in0=carry.t[:, tt - 1, :], in1=tot.t[:, tt - 1, :], op=ALU.add),
                     r=[tot.b, carry.b], w=[carry.b])
            p.op(V, lambda e: e.tensor_tensor(out=pos.t[:].rearrange("p t e -> p (t e)"), in0=psP.t[:],
                                              in1=carry.t[:].rearrange("p t e -> p (t e)"), op=ALU.add), r=[psP.b, carry.b], w=[pos.b])
            p.op(V, lambda e: e.tensor_scalar(out=ecap.t[:], in0=self.cst.t[:, CST['iota32']:CST['iota32'] + NE], scalar1=float(CAP),
                                              scalar2=None, op0=ALU.mult), r=[self.cst.b], w=[ecap.b])
            p.op(V, lambda e: e.tensor_tensor(out=pose.t[:], in0=pos.t[:], in1=ecap.t[:].unsqueeze(1).to_broadcast([128, NT, NE]), op=ALU.add),
                 r=[pos.b, ecap.b], w=[pose.b])
            for k in range(2):
                p.op(V, lambda e: e.tensor_tensor(out=tmp48.t[:], in0=Mk[k].t[:], in1=pos.t[:], op=ALU.mult), r=[Mk[k].b, pos.b], w=[tmp48.b])
                p.op(V, lambda e: e.tensor_reduce(out=posk.t[:], in_=tmp48.t[:], axis=AX.X, op=ALU.add), r=[tmp48.b], w=[posk.b])
                p.op(V, lambda e: e.tensor_tensor(out=tmp48.t[:], in0=Mk[k].t[:], in1=pose.t[:], op=ALU.mult), r=[Mk[k].b, pose.b], w=[tmp48.b])
                p.op(V, lambda e: e.tensor_reduce(out=dk.t[:], in_=tmp48.t[:], axis=AX.X, op=ALU.add), r=[tmp48.b], w=[dk.b])
                p.op(V, lambda e: e.tensor_single_scalar(out=valid.t[:], in_=posk.t[:], scalar=float(CAP), op=ALU.is_lt), r=[posk.b], w=[valid.b])
                p.op(V, lambda e: e.tensor_tensor(out=self.gate[k].t[:], in0=self.gate[k].t[:], in1=valid.t[:], op=ALU.mult),
                     r=[self.gate[k].b, valid.b], w=[self.gate[k].b])
                p.op(V, lambda e: e.scalar_tensor_tensor(out=dk.t[:], in0=dk.t[:], scalar=-float(NR), in1=valid.t[:], op0=ALU.add, op1=ALU.mult),
                     r=[dk.b, valid.b], w=[dk.b])
                p.op(V, lambda e: e.tensor_scalar(out=dk.t[:], in0=dk.t[:], scalar1=float(NR), scalar2=None, op0=ALU.add), r=[dk.b], w=[dk.b])
                p.op(V, lambda e: e.tensor_copy(out=self.dsti[k].t[:], in_=dk.t[:]), r=[dk.b], w=[self.dsti[k].b])
            self.dump('gate0', self.gate[0], [128, NT], F32)
            self.dump('gate1', self.gate[1], [128, NT], F32)
            self.dump('dst0', self.dsti[0], [128, NT], I32)
            self.dump('dst1', self.dsti[1], [128, NT], I32)
            p.barrier()
        with ExitStack() as les:
            NBR = 4
            ht = [self.sb(les, "htR%d" % i, [128, D], F32) for i in range(NBR)]
            hb = [self.sb(les, "hbR%d" % i, [128, D], BF16) for i in range(NBR)]

            def loadR(tt):
                p.dma('sp', [(ht[tt % NBR].t[:], self.h2S[tt * 128:(tt + 1) * 128, :])], r=[self.h2Sb], w=[ht[tt % NBR].b])
            for tt in range(NBR - 1):
                loadR(tt)
            for tt in range(NT):
                b = tt % NBR
                if tt + NBR - 1 < NT:
                    loadR(tt + NBR - 1)
                p.op('dve', lambda e: e.scalar_tensor_tensor(out=hb[b].t[:], in0=ht[b].t[:], scalar=self.rstd2.t[:, tt:tt + 1],
                                                             in1=self.gmoe_row.t[:], op0=ALU.mult, op1=ALU.mult),
                     r=[ht[b].b, self.rstd2.b, self.gmoe_row.b], w=[hb[b].b])
                p.dma_custom('pool', lambda e, k: e.indirect_dma_start(
                    out=self.xsS[:, :], out_offset=bass.IndirectOffsetOnAxis(ap=self.dsti[k].t[:, tt:tt + 1], axis=0),
                    in_=hb[b].t[:, :], in_offset=None), 2,
                    r=[hb[b].b, self.dsti[0].b, self.dsti[1].b, self.xs_zero], w=[self.xs_t[tt % 4]])
            p.barrier()

        if 'xs' in self.debug:
            o = nc.dram_tensor("dbg_xs", [NR, D], BF16, kind="ExternalOutput").ap()
            ob = p.buf("dbg_xs")
            p.dma('sp', [(o, self.xsS[0:NR, :])], r=self.xs_t, w=[ob])
            self.dbg_out['xs'] = ob

    def phaseG(self):
        nc, p, I = self.nc, self.p, self.I
        self.ys_e = [p.buf("ys_e%d" % i) for i in range(4)]
        CTS = [(i * 128, min(128, CAP - i * 128)) for i in range((CAP + 127) // 128)]
        NCT = len(CTS)
        with ExitStack() as les:
            wgu = [self.sb(les, "wgu%d" % i, [128, KC, 2 * DE], BF16) for i in range(2)]
            wdn = [self.sb(les, "wdn%d" % i, [128, DE // 128, D], BF16) for i in range(2)]
            xe = [self.sb(les, "xe%d" % i, [128, NCT, D], BF16) for i in range(2)]
            XeT = [self.sb(les, "XeT%d" % i, [128, KC, CAP], BF16) for i in range(2)]
            actT = [self.sb(les, "actT%d" % i, [128, DE // 128, CAP], BF16) for i in range(2)]
            sg = [self.sb(les, "sg%d" % i, [128, CAP], F32) for i in range(2)]
            ye = [self.sb(les, "ye%d" % i, [128, D], BF16) for i in range(2)]
            pX = self.ps(les, "pX", [128, D], BF16)
            pG = [self.ps(les, "pG%d" % i, [128, 512]) for i in range(2)]
            pU = [self.ps(les, "pU%d" % i, [128, 512]) for i in range(2)]
            pY = [self.ps(les, "pY%d" % i, [128, 512]) for i in range(2)]
            yi = 0

            def loadG(ex):
                b = ex % 2
                pre = ex < NPRE
                sgu = self.wguB if pre else I['w_gu']
                sdn = self.wdnB if pre else I['w_dn']
                rdep = [self.wB_g[ex // 8]] if pre else []
                p.dma('pool', [(wgu[b].t[:, q4 * 4:(q4 + 1) * 4, :],
                                sgu[ex * D + q4 * 512:ex * D + (q4 + 1) * 512, :].rearrange("(k p) c -> p k c", p=128))
                               for q4 in range(4)], r=rdep, w=[wgu[b].b])
                p.dma('pool', [(wdn[b].t[:, :, cg * 1024:(cg + 1) * 1024],
                                sdn[ex * DE:(ex + 1) * DE, cg * 1024:(cg + 1) * 1024].rearrange("(k p) c -> p k c", p=128))
                               for cg in range(2)], r=rdep, w=[wdn[b].b])
                p.dma('sp', [(xe[b].t[0:rows, ct, :], self.xsS[ex * CAP + r0:ex * CAP + r0 + rows, :]) for ct, (r0, rows) in enumerate(CTS)],
                      r=[self.xs_zero] + self.xs_t, w=[xe[b].b])

            def transG(ex):
                b = ex % 2
                for ct, (r0, rows) in enumerate(CTS):
                    for kc in range(KC):
                        p.op('pe', lambda e: e.transpose(pX.t[:, kc * 128:kc * 128 + rows], xe[b].t[0:rows, ct, kc * 128:(kc + 1) * 128],
                                                         self.ident_bf.t[0:rows, 0:rows]),
                             r=[xe[b].b, self.ident_bf.b], w=[pX.b], signal=(kc == KC - 1))
                    if ct % 2 == 0:
                        p.op('act', lambda e: e.activation(out=XeT[b].t[:, :, r0:r0 + rows],
                                                           in_=pX.t[:].rearrange("p (k t) -> p k t", k=KC)[:, :, 0:rows], func=AF.Copy),
                             r=[pX.b], w=[XeT[b].b])
                    else:
                        p.op('dve', lambda e: e.tensor_copy(out=XeT[b].t[:, :, r0:r0 + rows],
                                                            in_=pX.t[:].rearrange("p (k t) -> p k t", k=KC)[:, :, 0:rows]),
                             r=[pX.b], w=[XeT[b].b])

            loadG(0)
            loadG(1)
            transG(0)
            for ex in range(NE):
                b = ex % 2
                for fc in range(DE // 128):
                    fb = fc % 2
                    for kc in range(KC):
                        p.op('pe', lambda e: e.matmul(pG[fb].t[:, 0:CAP], wgu[b].t[:, kc, fc * 128:(fc + 1) * 128], XeT[b].t[:, kc, :],
                                                      start=(kc == 0), stop=(kc == KC - 1)),
                             r=[wgu[b].b, XeT[b].b], w=[pG[fb].b], signal=(kc == KC - 1))
                    for kc in range(KC):
                        p.op('pe', lambda e: e.matmul(pU[fb].t[:, 0:CAP], wgu[b].t[:, kc, DE + fc * 128:DE + (fc + 1) * 128], XeT[b].t[:, kc, :],
                                                      start=(kc == 0), stop=(kc == KC - 1)),
                             r=[wgu[b].b, XeT[b].b], w=[pU[fb].b], signal=(kc == KC - 1))
                    p.op('act', lambda e: e.activation(out=sg[fb].t[:], in_=pG[fb].t[:, 0:CAP], func=AF.Silu), r=[pG[fb].b], w=[sg[fb].b])
                    p.op('dve', lambda e: e.tensor_tensor(out=actT[b].t[:, fc, :], in0=pU[fb].t[:, 0:CAP], in1=sg[fb].t[:], op=ALU.mult),
                         r=[pU[fb].b, sg[fb].b], w=[actT[b].b])
                if ex + 1 < NE:
                    transG(ex + 1)
                for ct, (c0, rows) in enumerate(CTS):
                    yb_ = (ex * NCT + ct) % 2
                    for cg in range(4):
                        pb = cg % 2
                        for fc in range(DE // 128):
                            p.op('pe', lambda e: e.matmul(pY[pb].t[0:rows, :], actT[b].t[:, fc, c0:c0 + rows], wdn[b].t[:, fc, cg * 512:(cg + 1) * 512],
                                                          start=(fc == 0), stop=(fc == DE // 128 - 1)),
                                 r=[actT[b].b, wdn[b].b], w=[pY[pb].b], signal=(fc == DE // 128 - 1))
                        if cg % 2 == 0:
                            p.op('act', lambda e: e.activation(out=ye[yb_].t[0:rows, cg * 512:(cg + 1) * 512], in_=pY[pb].t[0:rows, :], func=AF.Copy),
                                 r=[pY[pb].b], w=[ye[yb_].b])
                        else:
                            p.op('dve', lambda e: e.tensor_copy(out=ye[yb_].t[0:rows, cg * 512:(cg + 1) * 512], in_=pY[pb].t[0:rows, :]),
                                 r=[pY[pb].b], w=[ye[yb_].b])
                    r0 = ex * CAP + c0
                    p.dma('sp', [(self.ysS[r0:r0 + rows, :], ye[yb_].t[0:rows, :])], r=[ye[yb_].b], w=[self.ys_e[yi % 4]])
                    yi += 1
                if ex + 2 < NE:
                    loadG(ex + 2)
            p.barrier()

    def phaseH(self):
        nc, p, I = self.nc, self.p, self.I
        NR = NE * CAP
        with ExitStack() as les:
            NBH = 4
            ht = [self.sb(les, "htH%d" % i, [128, D], F32) for i in range(NBH)]
            y1 = [self.sb(les, "y1H%d" % i, [128, D], BF16) for i in range(NBH)]
            y2 = [self.sb(les, "y2H%d" % i, [128, D], BF16) for i in range(NBH)]

            def loadH(tt):
                b = tt % NBH
                p.dma('sp', [(ht[b].t[:], self.h2S[tt * 128:(tt + 1) * 128, :])], r=[self.h2Sb], w=[ht[b].b])
                for (yy, k) in ((y1, 0), (y2, 1)):
                    p.dma_custom('pool', lambda e, _k: e.indirect_dma_start(
                        out=yy[b].t[:, :], out_offset=None, in_=self.ysS[:, :],
                        in_offset=bass.IndirectOffsetOnAxis(ap=self.dsti[k].t[:, tt:tt + 1], axis=0),
                        ), 1,
                        r=[self.dsti[k].b, self.ys_zero] + self.ys_e, w=[yy[b].b])
            for tt in range(NBH - 1):
                loadH(tt)
            for tt in range(NT):
                b = tt % NBH
                if tt + NBH - 1 < NT:
                    loadH(tt + NBH - 1)
                p.op('dve', lambda e: e.scalar_tensor_tensor(out=ht[b].t[:], in0=y1[b].t[:], scalar=self.gate[0].t[:, tt:tt + 1],
                                                             in1=ht[b].t[:], op0=ALU.mult, op1=ALU.add),
                     r=[y1[b].b, self.gate[0].b, ht[b].b], w=[ht[b].b])
                p.op('dve', lambda e: e.scalar_tensor_tensor(out=ht[b].t[:], in0=y2[b].t[:], scalar=self.gate[1].t[:, tt:tt + 1],
                                                             in1=ht[b].t[:], op0=ALU.mult, op1=ALU.add),
                     r=[y2[b].b, self.gate[1].b, ht[b].b], w=[ht[b].b])
                p.dma('sp', [(self.y[tt * 128:(tt + 1) * 128, :], ht[b].t[:])], r=[ht[b].b], w=[self.yb])

    def phaseB_attn(self):
        nc, p, I = self.nc, self.p, self.I
        es = self.es
        self.oT = self.sb(es, "oT", [128, 8, T], BF16)
        oT = self.oT
        xnT = self.xnT
        with ExitStack() as les:
            kTz = self.sb(les, "kTz", [128, 2, 2, T], BF16)
            v = self.sb(les, "v", [128, NT, 128], BF16)
            bias = self.sb(les, "bias", [128, NQH, 256], BF16)
            esink = self.sb(les, "esink", [128, NQH], F32)
            esc = self.sb(les, "esc", [128, 8], F32)
            qgs = self.sb(les, "qgs", [128, 1], F32)
            wq = [self.sb(les, "wq%d" % i, [128, KC, 128], BF16) for i in range(2)]
            qc = [self.sb(les, "qc%d" % i, [128, T + 128], BF16) for i in range(2)]
            sqb1 = self.sb(les, "sqb", [128, 512], BF16)
            sqb = [sqb1, sqb1]
            rr = [self.sb(les, "rr%d" % i, [128, 512], F32) for i in range(2)]
            pT = [self.sb(les, "pT%d" % i, [128, NT, 256], BF16) for i in range(2)]
            rden = self.sb(les, "rden", [128, 512], F32)
            psQ = [self.ps(les, "psQ%d" % i, [128, 512]) for i in range(2)]
            psS = self.ps(les, "psS", [128, 512])
            psSc = [self.ps(les, "psSc%d" % i, [128, 2, 256]) for i in range(3)]
            psO = self.ps(les, "psO", [128, 512])
            psD = self.ps(les, "psD", [128, 512])
            p.dma('pool', [(bias.t[:], I['biasT'].rearrange("p (h c) -> p h c", h=NQH))], w=[bias.b])
            mask = self.cst.t[:, CST['mask']:CST['mask'] + 256]
            p.op('dve', lambda e: e.tensor_tensor(out=bias.t[:], in0=bias.t[:], in1=mask.unsqueeze(1).to_broadcast([128, NQH, 256]),
                                                  op=ALU.add), r=[bias.b, self.cst.b], w=[bias.b])
            p.op('act', lambda e: e.activation(out=esink.t[:], in_=self.prow.t[:, PROW['sinks']:PROW['sinks'] + NQH], func=AF.Exp),
                 r=[self.prow.b], w=[esink.b])
            es2 = esink.t[:].rearrange("p (c two) -> p c two", two=2)
            p.op('dve', lambda e: e.tensor_copy(out=esc.t[0:64, :], in_=es2[0:64, :, 0]), r=[esink.b], w=[esc.b])
            p.op('dve', lambda e: e.tensor_copy(out=esc.t[64:128, :], in_=es2[64:128, :, 1]), r=[esink.b], w=[esc.b])
            p.op('dve', lambda e: e.tensor_scalar(out=qgs.t[:], in0=self.pc('qg'), scalar1=HD ** -0.5, scalar2=None, op0=ALU.mult),
                 r=[self.pcol.b], w=[qgs.b])
            p.op('dve', lambda e: e.memset(qc[0].t[:, T:T + 128], 0.0), w=[qc[0].b])
            p.op('dve', lambda e: e.memset(qc[1].t[:, T:T + 128], 0.0), w=[qc[1].b])
            self.load_w(wq[0].t[:], wq[0].b, I['w_in'][:, OFF_V:OFF_V + 128])
            for g4 in range(4):
                pq = psQ[g4 % 2]
                for ti in range(4):
                    tt = g4 * 4 + ti
                    for kc in range(KC):
                        p.op('pe', lambda e: e.matmul(pq.t[:, ti * 128:(ti + 1) * 128], xnT.t[:, kc, tt * 128:(tt + 1) * 128],
                                                      wq[0].t[:, kc, :], start=(kc == 0), stop=(kc == KC - 1)),
                             r=[wq[0].b, xnT.b], w=[pq.b], signal=(kc == KC - 1))
                p.op('act', lambda e: e.activation(out=v.t[:, g4 * 4:(g4 + 1) * 4, :], in_=pq.t[:].rearrange("p (a c) -> p a c", a=4),
                                                   func=AF.Copy), r=[pq.b], w=[v.b])
            for kvh in range(NKV):
                wk = wq[(kvh + 1) % 2]
                src = I['w_in'][:, OFF_K + kvh * HD:OFF_K + (kvh + 1) * HD].rearrange("(k p) c -> p k c", p=128)
                p.dma('pool', [(wk.t[:, :, 0:HD], src), (wk.t[:, :, HD:2 * HD], src)], w=[wk.b])
                self.precast_next(after=psQ[1].b)
                self.qk_chunk(wk, kTz.t[:, kvh, 0, :], kTz.b, self.pc('kg'), self.pcol.b, psQ, psS, sqb, rr)
                p.op('dve', lambda e: e.tensor_copy(out=kTz.t[64:128, kvh, 1, :], in_=kTz.t[64:128, kvh, 0, :]), r=[kTz.b], w=[kTz.b])
                p.op('dve', lambda e: e.memset(kTz.t[64:128, kvh, 0, :], 0.0), w=[kTz.b])
                p.op('dve', lambda e: e.memset(kTz.t[0:64, kvh, 1, :], 0.0), w=[kTz.b])
            def loadQ(c):
                b = c % 2
                self.load_w(wq[b].t[:], wq[b].b, I['w_in'][:, OFF_Q + c * 128:OFF_Q + (c + 1) * 128])
                self.precast_next(after=psO.b)
                self.zero_fill_next(after=psO.b)

            def qpart(c, n):
                b = c % 2
                self.qk_part(wq[b], qc[b].t, qc[b].b, qgs.t[:], qgs.b, psQ, psS, sqb, rr, n)
            loadQ(0)
            for n in range(4):
                qpart(0, n)
            for c in range(8):
                b = c % 2
                kvh = c // 4
                if c + 1 < 8:
                    loadQ(c + 1)
                for hp in range(2):
                    h = 2 * c + hp
                    pr = slice(hp * 64, (hp + 1) * 64)
                    for jp in range(8):
                        if c + 1 < 8 and jp % 4 == 0:
                            qpart(c + 1, hp * 2 + jp // 4)
                        sc = psSc[jp % 3]
                        for jj in range(2):
                            j = 2 * jp + jj
                            p.op('pe', lambda e: e.matmul(sc.t[:, jj, :], kTz.t[:, kvh, hp, j * 128:(j + 1) * 128],
                                                          qc[b].t[:, j * 128:j * 128 + 256], start=True, stop=False),
                                 r=[kTz.b, qc[b].b], w=[sc.b], signal=False)
                            p.op('pe', lambda e: e.matmul(sc.t[:, jj, :], self.ident_bf.t[:], bias.t[:, h, :], start=False, stop=True),
                                 r=[self.ident_bf.b, bias.b], w=[sc.b], signal=(jj == 1))
                        p.op('act', lambda e: e.activation(out=pT[hp].t[:, 2 * jp:2 * jp + 2, :], in_=sc.t[:], func=AF.Exp),
                             r=[sc.b], w=[pT[hp].b])
                for g4 in range(4):
                    for hp in range(2):
                        pr = slice(hp * 64, (hp + 1) * 64)
                        vs = slice(kvh * HD, (kvh + 1) * HD)
                        for (pso, get_l) in ((psO, lambda n: v.t[:, n, vs]), (psD, lambda n: self.ones_bf.t[:, 0:HD])):
                            for nn in range(4):
                                n = g4 * 4 + nn
                                cs = slice(nn * 128, (nn + 1) * 128)
                                if n > 0:
                                    p.op('pe', lambda e: e.matmul(pso.t[pr, cs], get_l(n - 1), pT[hp].t[:, n - 1, 128:256],
                                                                  start=True, stop=False),
                                         r=[v.b, self.ones_bf.b, pT[hp].b], w=[pso.b], signal=False)
                                p.op('pe', lambda e: e.matmul(pso.t[pr, cs], get_l(n), pT[hp].t[:, n, 0:128],
                                                              start=(n == 0), stop=True),
                                     r=[v.b, self.ones_bf.b, pT[hp].b], w=[pso.b], signal=(nn == 3))
                    p.op('dve', lambda e: e.tensor_scalar(out=rden.t[:], in0=psD.t[:], scalar1=esc.t[:, c:c + 1], scalar2=None, op0=ALU.add),
                         r=[psD.b, esc.b], w=[rden.b])
                    p.op('dve', lambda e: e.reciprocal(out=rden.t[:], in_=rden.t[:]), r=[rden.b], w=[rden.b])
                    p.op('dve', lambda e: e.tensor_tensor(out=oT.t[:, c, g4 * 512:(g4 + 1) * 512], in0=psO.t[:], in1=rden.t[:], op=ALU.mult),
                         r=[psO.b, rden.b], w=[oT.b])
            p.barrier()
        self.dump('oT', oT, [128, 8, T], BF16)


PCOL = {}
PROW = {}
CST = {}


def _mk_layout():
    o = 0
    for name, n in [('gmix', KC), ('gx', KC), ('gmem', KC), ('gmoe', KC), ('dww', 8 * CW), ('dwb', 8), ('lng', 8),
                    ('lnb', 8), ('qg', 1), ('kg', 1), ('xkg', 1)]:
        PCOL[name] = o
        o += n
    global PCOL_N
    PCOL_N = o
    o = 0
    for name, n in [('sinks', NQH), ('xqg', XW), ('rtb', 36)]:
        PROW[name] = o
        o += n
    global PROW_N
    PROW_N = o
    o = 0
    for name, n in [('ident', 128), ('blk', 128), ('tri', 128), ('mask', 256), ('eps', 1), ('iota32', NE)]:
        CST[name] = o
        o += n
    global CST_N
    CST_N = o


PCOL_N = PROW_N = CST_N = 0
_mk_layout()


def _t5_bucket_np(dist):
    n = np.maximum(dist, 0)
    max_exact = 16
    large = max_exact + (np.log(np.maximum(n, 1).astype(np.float32) / max_exact)
                         / np.float32(np.log(128 / max_exact)) * (32 - max_exact)).astype(np.int32)
    large = np.minimum(large, 31)
    return np.where(n < max_exact, n, large)


def host_pack(inp):
    f = np.float32
    colT = lambda v: np.ascontiguousarray(v.reshape(-1, 128).T)
    pcol = np.zeros((128, PCOL_N), f)

    def put(name, arr):
        pcol[:, PCOL[name]:PCOL[name] + arr.shape[1]] = arr

    put('gmix', colT(inp['norm_mix_g'][0]))
    put('gx', colT(inp['norm_x_g'][0]))
    put('gmem', colT(inp['norm_mem_g'][0]))
    put('gmoe', colT(inp['norm_moe_g'][0]))
    dw = inp['conv_dw_w'][0][:, 0, :]
    put('dww', np.ascontiguousarray(dw.reshape(CW, 8, 128).transpose(2, 1, 0).reshape(128, 8 * CW)))
    put('dwb', colT(inp['conv_dw_b'][0]))
    put('lng', colT(inp['conv_ln_g'][0]))
    put('lnb', colT(inp['conv_ln_b'][0]))
    put('qg', np.tile(inp['q_norm_g'][0], 2).reshape(128, 1))
    put('kg', np.tile(inp['k_norm_g'][0], 2).reshape(128, 1))
    put('xkg', inp['xk_norm_g'][0].reshape(128, 1))
    prow = np.zeros((128, PROW_N), f)

    def putr(name, vec):
        prow[:, PROW[name]:PROW[name] + vec.shape[0]] = np.broadcast_to(vec[None, :], (128, vec.shape[0]))

    putr('sinks', inp['attn_sinks'][0])
    putr('xqg', np.tile(inp['xq_norm_g'][0], XH))
    putr('rtb', np.concatenate([inp['b_router_group'][0], inp['b_router_expert'][0]]))
    cst = np.zeros((128, CST_N), f)
    cst[:, CST['ident']:CST['ident'] + 128] = np.eye(128, dtype=f)
    blk = np.zeros((128, 128), f)
    blk[:64, :64] = 1
    blk[64:, 64:] = 1
    cst[:, CST['blk']:CST['blk'] + 128] = blk
    cst[:, CST['tri']:CST['tri'] + 128] = np.triu(np.ones((128, 128), f), 1)
    kj = np.arange(128)[:, None]
    qi = np.arange(128)[None, :]
    mask = np.zeros((128, 256), f)
    mask[:, :128] = np.where(qi - kj >= 0, 0.0, NEGM)
    mask[:, 128:] = np.where(kj > qi, 0.0, NEGM)
    cst[:, CST['mask']:CST['mask'] + 256] = mask
    cst[:, CST['eps']] = EPS
    cst[:, CST['iota32']:CST['iota32'] + NE] = np.arange(NE, dtype=f)[None, :]
    dist = np.concatenate([np.maximum(qi - kj, 0), np.clip(qi + 128 - kj, 0, 127)], axis=1)
    bk = _t5_bucket_np(dist)
    biasT = np.ascontiguousarray(inp['rel_bias'][bk].transpose(0, 2, 1)).reshape(128, NQH * 256).astype(f)
    w_rt = np.ascontiguousarray(np.concatenate([inp['w_router_group'][0], inp['w_router_expert'][0]], axis=1))
    shared = {
        'w_in': inp['w_in'][0], 'w_conv_out': inp['w_conv_out'][0], 'w_attn_out': inp['w_attn_out'][0],
        'w_mix_out': inp['w_mix_out'][0], 'w_xq': inp['w_xq'][0], 'w_xkv': inp['w_xkv'][0], 'w_xo': inp['w_xo'][0],
        'w_gu': inp['w_expert_gu'][0].reshape(NE * D, 2 * DE), 'w_dn': inp['w_expert_down'][0].reshape(NE * DE, D),
        'w_rt': w_rt, 'zsrc': np.zeros((640, D // 2), np.float32), 'gmoe_row': np.ascontiguousarray(np.broadcast_to(inp['norm_moe_g'][0][None, :], (128, D))), 'pcol': pcol, 'prow': prow, 'biasT': biasT, 'cst': cst,
    }
    return shared


def kernel(**inp):
    inp = {k: np.asarray(v) for k, v in inp.items()}
    shared = host_pack(inp)
    B = inp['x'].shape[0]
    k = Kern()
    nc = k.build()
    in_maps = []
    for b in range(B):
        m = dict(shared)
        m['x'] = np.ascontiguousarray(inp['x'][b])
        m['mem'] = np.ascontiguousarray(inp['mem'][b])
        in_maps.append(m)
    res = run_bass_kernel_spmd(nc, in_maps, core_ids=list(range(B)))
    return np.stack([np.asarray(r["y"]) for r in res.results], axis=0).astype(np.float32)
```

```python
import numpy as np
from contextlib import ExitStack
import concourse.bass as bass
import concourse.mybir as mybir
from concourse.bass_utils import run_bass_kernel_spmd

F32 = mybir.dt.float32
BF16 = mybir.dt.bfloat16
I32 = mybir.dt.int32
U32 = mybir.dt.uint32
AF = mybir.ActivationFunctionType
ALU = mybir.AluOpType
AX = mybir.AxisListType

D = 2048
T = 2048
NT = T // 128
KC = D // 128
MEM = 256
CONV_CH = 1024
CW = 31
NQH = 16
NKV = 2
HD = 64
ATT = 1024
XH = 4
XHD = 128
XW = 512
NG = 4
EPG = 8
NE = 32
DE = 512
IN_W = 7424
EPS = 1e-6
CAP = 288
NEGM = -30000.0
NPRE = 24
OFF_CA, OFF_CB, OFF_Q, OFF_K, OFF_V, OFF_G = 0, 1024, 2048, 3072, 3200, 3328

SAME_ENG_SYNC = True


class Buf:
    def __init__(self, name):
        self.name = name
        self.w = None
        self.r = {}
        self.dsem = None
        self.dcnt = 0


class Prog:
    def __init__(self, nc, es):
        self.nc = nc
        self.es = ExitStack()
        self.engs = {'pe': nc.tensor, 'dve': nc.vector, 'act': nc.scalar, 'pool': nc.gpsimd, 'sp': nc.sync}
        self.esem = {k: self.es.enter_context(nc.semaphore('e_' + k)) for k in self.engs}
        self.ecnt = {k: 0 for k in self.engs}
        self.waited = {}
        self.nsem = len(self.engs)
        self.nbuf = 0
        self.dbufs = []

    def buf(self, name=None):
        self.nbuf += 1
        return Buf(name or ('b%d' % self.nbuf))

    def _wait(self, eng, tok):
        sem, val, key = tok
        if key == 'e_' + eng and (eng in ('pe', 'sp') or not SAME_ENG_SYNC):
            return
        if self.waited.get((eng, key), 0) >= val:
            return
        self.engs[eng].wait_ge(sem, val)
        self.waited[(eng, key)] = val

    def _deps(self, eng, r, w):
        for b in r:
            if b.w is not None:
                self._wait(eng, b.w)
        for b in w:
            if b.w is not None:
                self._wait(eng, b.w)
            for t in b.r.values():
                self._wait(eng, t)

    def _mark(self, tok, r, w):
        for b in r:
            old = b.r.get(tok[2])
            if old is None or old[1] < tok[1]:
                b.r[tok[2]] = tok
        for b in w:
            b.w = tok
            b.r = {}

    def op(self, eng, fn, r=(), w=(), signal=True):
        self._deps(eng, r, w)
        ins = fn(self.engs[eng])
        if signal:
            self.ecnt[eng] += 1
            ins.then_inc(self.esem[eng], 1)
            tok = (self.esem[eng], self.ecnt[eng], 'e_' + eng)
        else:
            tok = (self.esem[eng], self.ecnt[eng] + 1, 'e_' + eng)
        self._mark(tok, r, w)
        return ins

    def dma(self, q, pairs, r=(), w=(), **kw):
        self._deps(q, r, w)
        wb = w[0]
        if wb.dsem is None:
            wb.dsem = self.es.enter_context(self.nc.semaphore('d_' + wb.name))
            self.nsem += 1
        for (o, i) in pairs:
            self.engs[q].dma_start(out=o, in_=i, **kw).then_inc(wb.dsem, 16)
            wb.dcnt += 16
        tok = (wb.dsem, wb.dcnt, 'd_' + wb.name)
        if wb not in self.dbufs:
            self.dbufs.append(wb)
        self._mark(tok, r, w)

    def dma_nowait(self, q, pairs, wb):
        if wb.dsem is None:
            wb.dsem = self.es.enter_context(self.nc.semaphore('d_' + wb.name))
            self.nsem += 1
        for (o, i) in pairs:
            self.engs[q].dma_start(out=o, in_=i).then_inc(wb.dsem, 16)
            wb.dcnt += 16
        wb.w = (wb.dsem, wb.dcnt, 'd_' + wb.name)
        if wb not in self.dbufs:
            self.dbufs.append(wb)

    def dma_custom(self, q, fn, n, r=(), w=()):
        self._deps(q, r, w)
        wb = w[0]
        if wb.dsem is None:
            wb.dsem = self.es.enter_context(self.nc.semaphore('d_' + wb.name))
            self.nsem += 1
        for k in range(n):
            fn(self.engs[q], k).then_inc(wb.dsem, 16)
            wb.dcnt += 16
        tok = (wb.dsem, wb.dcnt, 'd_' + wb.name)
        if wb not in self.dbufs:
            self.dbufs.append(wb)
        self._mark(tok, r, w)

    def barrier(self):
        for e in self.engs:
            for o in self.engs:
                if o != e and self.ecnt[o] > 0:
                    self._wait(e, (self.esem[o], self.ecnt[o], 'e_' + o))
            for b in self.dbufs:
                self._wait(e, (b.dsem, b.dcnt, 'd_' + b.name))
        self.dbufs = []

    def wait_buf(self, eng, b):
        if b.w is not None:
            self._wait(eng, b.w)


class TB:
    def __init__(self, t, b):
        self.t = t
        self.b = b


class Kern:
    def __init__(self, stop_after=None, debug=()):
        self.stop_after = stop_after
        self.debug = set(debug)
        self.nc = bass.Bass("TRN2", target_bir_lowering=False)
        self.dbg_out = {}

    def sb(self, es, name, shape, dt):
        t = es.enter_context(self.nc.sbuf_tensor("s_" + name, shape, dt))
        return TB(t, self.p.buf(name))

    def ps(self, es, name, shape, dt=F32):
        t = es.enter_context(self.nc.psum_tensor("p_" + name, shape, dt))
        return TB(t, self.p.buf(name))

    def din(self, name, shape, dt=F32):
        return self.nc.dram_tensor(name, list(shape), dt, kind="ExternalInput").ap()

    def dump(self, name, tb, shape, dt=F32):
        if name not in self.debug:
            return
        o = self.nc.dram_tensor("dbg_" + name, list(shape), dt, kind="ExternalOutput").ap()
        ob = self.p.buf("dbg_" + name)
        self.p.dma('sp', [(o, tb.t[:])], r=[tb.b], w=[ob])
        self.dbg_out[name] = ob

    def load_w(self, dst, dstb, src_rows_cols, q='pool'):
        self.p.dma(q, [(dst, src_rows_cols.rearrange("(k p) c -> p k c", p=128))], w=[dstb])

    def build(self):
        nc = self.nc
        es = ExitStack()
        self.es = es
        self.p = Prog(nc, es)
        p = self.p
        I = {}
        I['x'] = self.din('x', [T, D])
        I['mem'] = self.din('mem', [MEM, D])
        I['w_in'] = self.din('w_in', [D, IN_W])
        I['w_conv_out'] = self.din('w_conv_out', [CONV_CH, D])
        I['w_attn_out'] = self.din('w_attn_out', [ATT, D])
        I['w_mix_out'] = self.din('w_mix_out', [D, D])
        I['w_xq'] = self.din('w_xq', [D, XW])
        I['w_xkv'] = self.din('w_xkv', [D, 2 * XW])
        I['w_xo'] = self.din('w_xo', [XW, D])
        I['w_gu'] = self.din('w_gu', [NE * D, 2 * DE])
        I['w_dn'] = self.din('w_dn', [NE * DE, D])
        I['w_rt'] = self.din('w_rt', [D, 36])
        I['pcol'] = self.din('pcol', [128, PCOL_N])
        I['prow'] = self.din('prow', [128, PROW_N])
        I['biasT'] = self.din('biasT', [128, NQH * 256])
        I['cst'] = self.din('cst', [128, CST_N])
        I['gmoe_row'] = self.din('gmoe_row', [128, D])
        I['zsrc'] = self.din('zsrc', [640, D // 2])
        self.I = I
        self.y = nc.dram_tensor("y", [T, D], F32, kind="ExternalOutput").ap()
        self.yb = p.buf('y')

        ces = ExitStack()
        self.ces = ces
        self.consts(ces)
        self.precast_init()
        with es:
            self.phaseA()
            if self.stop_after == 'A':
                return self.finish()
            self.phaseM()
            self.phaseB_conv()
            if self.stop_after == 'Bc':
                return self.finish()
            self.phaseB_attn()
            if self.stop_after == 'Ba':
                return self.finish()
            self.phaseC()
            if self.stop_after == 'C':
                return self.finish()
        es = ExitStack()
        self.es = es
        with es:
            self.phaseD()
            if self.stop_after == 'D':
                return self.finish()
            self.phaseR()
            if self.stop_after == 'R':
                return self.finish()
            self.phaseG()
            if self.stop_after == 'G':
                return self.finish()
            self.phaseH()
            return self.finish()
            return self.finish()

    def finish(self):
        p = self.p
        for ob in list(self.dbg_out.values()) + [self.yb]:
            if ob.w is not None:
                p._wait('sp', ob.w)
        return self.nc

    def consts(self, es):
        nc, p, I = self.nc, self.p, self.I
        self.pcol = self.sb(es, "pcol", [128, PCOL_N], F32)
        self.prow = self.sb(es, "prow", [128, PROW_N], F32)
        self.cst = self.sb(es, "cst", [128, CST_N], F32)
        p.dma('sp', [(self.pcol.t[:], I['pcol'])], w=[self.pcol.b])
        p.dma('sp', [(self.prow.t[:], I['prow'])], w=[self.prow.b])
        p.dma('sp', [(self.cst.t[:], I['cst'])], w=[self.cst.b])
        self.ident_bf = self.sb(es, "ident_bf", [128, 128], BF16)
        self.ident_f = TB(self.cst.t, self.cst.b)
        p.op('dve', lambda e: e.tensor_copy(out=self.ident_bf.t[:], in_=self.cst.t[:, CST['ident']:CST['ident'] + 128]),
             r=[self.cst.b], w=[self.ident_bf.b])
        self.ones_bf = self.sb(es, "ones_bf", [128, 128], BF16)
        p.op('dve', lambda e: e.memset(self.ones_bf.t[:], 1.0), w=[self.ones_bf.b])
        self.ones_f = self.sb(es, "ones_f", [128, 128], F32)
        p.op('dve', lambda e: e.memset(self.ones_f.t[:], 1.0), w=[self.ones_f.b])
        self.kxT = self.sb(es, "kxT", [128, XH, MEM], BF16)
        self.vx = self.sb(es, "vx", [128, 2, XW], BF16)
        self.blk_bf = self.sb(es, "blk_bf", [128, 128], BF16)
        p.op('dve', lambda e: e.tensor_copy(out=self.blk_bf.t[:], in_=self.cst.t[:, CST['blk']:CST['blk'] + 128]),
             r=[self.cst.b], w=[self.blk_bf.b])

    def precast_init(self):
        nc, p = self.nc, self.p
        self.wguB = nc.dram_tensor("wguB", [NE * 128, KC * 2 * DE], BF16, kind="Internal").ap()
        self.wdnB = nc.dram_tensor("wdnB", [NE * 128, (DE // 128) * D], BF16, kind="Internal").ap()
        self.wB_g = [p.buf("wB_g%d" % i) for i in range(4)]
        self.precast_i = 0
        NR = NE * CAP
        self.xsS = nc.dram_tensor("xsS", [NR + 1, D], BF16, kind="Internal").ap()
        self.xs_zero = p.buf("xs_zero")
        self.zf_r0 = 0

    def zero_fill_next(self, n=1, after=None):
        p = self.p
        NR = NE * CAP
        xsf = self.xsS.bitcast(F32)
        if after is not None and after.w is not None and self.zf_r0 < NR + 1:
            p._wait('sp', after.w)
        for _ in range(n):
            r0 = self.zf_r0
            if r0 >= NR + 1:
                return
            m = min(640, NR + 1 - r0)
            self.zf_r0 += m
            p.dma_nowait('sp', [(xsf[r0:r0 + m, :], self.I['zsrc'][0:m, :])], self.xs_zero)

    def precast_next(self, n=1, after=None):
        p, I = self.p, self.I
        if after is not None and after.w is not None and self.precast_i < NPRE:
            p._wait('pool', after.w)
        for _ in range(n):
            ex = self.precast_i
            if ex >= NPRE:
                return
            self.precast_i += 1
            pairs = []
            for hh in range(2):
                r0 = ex * D + hh * 1024
                pairs.append((self.wguB[ex * 128:(ex + 1) * 128, hh * 8 * 1024:(hh + 1) * 8 * 1024].rearrange("p (k c) -> p k c", k=8),
                              I['w_gu'][r0:r0 + 1024, :].rearrange("(k p) c -> p k c", p=128)))
            pairs.append((self.wdnB[ex * 128:(ex + 1) * 128, :].rearrange("p (k c) -> p k c", k=DE // 128),
                          I['w_dn'][ex * DE:(ex + 1) * DE, :].rearrange("(k p) c -> p k c", p=128)))
            p.dma_nowait('pool', pairs, self.wB_g[ex // 8])

    def pc(self, name, i=0, n=1):
        o = PCOL[name] + i
        return self.pcol.t[:, o:o + n]

    def rms_stats(self, src, srcb, junk, ssq, sd, rstd):
        p = self.p
        p.op('act', lambda e: e.activation(out=junk.t[:], in_=src, func=AF.Square, accum_out=ssq.t[:]),
             r=[srcb], w=[junk.b, ssq.b])
        p.op('act', lambda e: e.activation(out=sd.t[:], in_=ssq.t[:], func=AF.Sqrt, bias=self.epsc(), scale=1.0 / D),
             r=[ssq.b, self.cst.b], w=[sd.b])
        p.op('dve', lambda e: e.reciprocal(out=rstd.t[:], in_=sd.t[:]), r=[sd.b], w=[rstd.b])

    def epsc(self):
        return self.cst.t[:, CST['eps']:CST['eps'] + 1]

    def phaseA(self):
        nc, p, I = self.nc, self.p, self.I
        es = self.es
        self.xnT = self.sb(es, "xnT", [128, KC, T], BF16)
        with ExitStack() as les:
            NBA = 4
            xt = [self.sb(les, "xtA%d" % i, [128, D], F32) for i in range(NBA)]
            xs = [self.sb(les, "xsA%d" % i, [128, D], BF16) for i in range(NBA)]
            junk = self.sb(les, "junkA", [128, D], BF16)
            ssq = [self.sb(les, "ssqA%d" % i, [128, 1], F32) for i in range(NBA)]
            sd = [self.sb(les, "sdA%d" % i, [128, 1], F32) for i in range(NBA)]
            rstd = [self.sb(les, "rstdA%d" % i, [128, 1], F32) for i in range(NBA)]
            pT2 = [self.ps(les, "pTA%d" % i, [128, D], BF16) for i in range(2)]
            pT = [pT2[i % 2] for i in range(NBA)]
            gmix = self.pcol.t[:, PCOL['gmix']:PCOL['gmix'] + KC]
            for tt in range(NBA - 1):
                p.dma('sp', [(xt[tt].t[:], I['x'][tt * 128:(tt + 1) * 128, :])], w=[xt[tt].b])
            for tt in range(NT):
                b = tt % NBA
                t2 = tt + NBA - 1
                if t2 < NT:
                    p.dma('sp', [(xt[t2 % NBA].t[:], I['x'][t2 * 128:(t2 + 1) * 128, :])], w=[xt[t2 % NBA].b])
                self.rms_stats(xt[b].t[:], xt[b].b, junk, ssq[b], sd[b], rstd[b])
                p.op('pool', lambda e: e.tensor_scalar(out=xs[b].t[:], in0=xt[b].t[:], scalar1=rstd[b].t[:], scalar2=0.0,
                                                       op0=ALU.mult, op1=ALU.add),
                     r=[xt[b].b, rstd[b].b], w=[xs[b].b])
                for kc in range(KC):
                    p.op('pe', lambda e: e.transpose(pT[b].t[:, kc * 128:(kc + 1) * 128], xs[b].t[:, kc * 128:(kc + 1) * 128],
                                                     self.ident_bf.t[:]),
                         r=[xs[b].b, self.ident_bf.b], w=[pT[b].b], signal=(kc == KC - 1))
                p.op('dve', lambda e: e.tensor_tensor(
                    out=self.xnT.t[:, :, tt * 128:(tt + 1) * 128],
                    in0=pT[b].t[:].rearrange("p (k t) -> p k t", k=KC),
                    in1=gmix.unsqueeze(2).to_broadcast([128, KC, 128]), op=ALU.mult),
                     r=[pT[b].b, self.pcol.b], w=[self.xnT.b])
            p.barrier()
        self.dump('xnT', self.xnT, [128, KC, T], BF16)

    def phaseB_conv(self):
        nc, p, I = self.nc, self.p, self.I
        es = self.es
        self.u = self.sb(es, "u", [128, 8, T + 30], BF16)
        u = self.u
        xnT = self.xnT
        p.op('dve', lambda e: e.memset(u.t[:], 0.0), w=[u.b])
        with ExitStack() as les:
            conv = self.sb(les, "conv", [128, 8, T], F32)
            S1 = self.ps(les, "S1", [128, 512])
            S2 = self.ps(les, "S2", [128, 512])
            lw = ExitStack()
            diag1 = self.sb(lw, "diag", [128, CW, 128], BF16)
            diag = [diag1, diag1]
            wca = [self.sb(lw, "wca%d" % i, [128, KC, 128], BF16) for i in range(2)]
            wcb = [self.sb(lw, "wcb%d" % i, [128, KC, 128], BF16) for i in range(2)]
            sig = [self.sb(lw, "sig%d" % i, [128, 512], F32) for i in range(2)]
            psA = [self.ps(lw, "psA%d" % i, [128, 512]) for i in range(2)]
            psB = [self.ps(lw, "psB%d" % i, [128, 512]) for i in range(2)]
            psC = [self.ps(lw, "psC%d" % i, [128, 512]) for i in range(2)]
            idf = self.cst.t[:, CST['ident']:CST['ident'] + 128]
            for c in range(8):
                b = c % 2
                self.load_w(wca[b].t[:], wca[b].b, I['w_in'][:, OFF_CA + c * 128:OFF_CA + (c + 1) * 128])
                self.load_w(wcb[b].t[:], wcb[b].b, I['w_in'][:, OFF_CB + c * 128:OFF_CB + (c + 1) * 128])
                self.precast_next(after=psC[1].b)
                self.zero_fill_next(after=psC[1].b)
                dww = self.pcol.t[:, PCOL['dww'] + c * CW:PCOL['dww'] + (c + 1) * CW]
                p.op('dve', lambda e: e.tensor_tensor(out=diag[b].t[:], in0=idf.unsqueeze(1).to_broadcast([128, CW, 128]),
                                                      in1=dww.unsqueeze(2).to_broadcast([128, CW, 128]), op=ALU.mult),
                     r=[self.cst.b, self.pcol.b], w=[diag[b].b])
                for n in range(4):
                    nb = n % 2
                    ns = slice(n * 512, (n + 1) * 512)
                    for kc in range(KC):
                        p.op('pe', lambda e: e.matmul(psA[nb].t[:], wca[b].t[:, kc, :], xnT.t[:, kc, ns],
                                                      start=(kc == 0), stop=(kc == KC - 1)),
                             r=[wca[b].b, xnT.b], w=[psA[nb].b], signal=(kc == KC - 1))
                    for kc in range(KC):
                        p.op('pe', lambda e: e.matmul(psB[nb].t[:], wcb[b].t[:, kc, :], xnT.t[:, kc, ns],
                                                      start=(kc == 0), stop=(kc == KC - 1)),
                             r=[wcb[b].b, xnT.b], w=[psB[nb].b], signal=(kc == KC - 1))
                    p.op('act', lambda e: e.activation(out=sig[nb].t[:], in_=psB[nb].t[:], func=AF.Sigmoid),
                         r=[psB[nb].b], w=[sig[nb].b])
                    p.op('dve', lambda e: e.tensor_tensor(out=u.t[:, c, 30 + n * 512:30 + (n + 1) * 512], in0=psA[nb].t[:],
                                                          in1=sig[nb].t[:], op=ALU.mult),
                         r=[psA[nb].b, sig[nb].b], w=[u.b])
                for n in range(4):
                    nb = n % 2
                    for k in range(CW):
                        p.op('pe', lambda e: e.matmul(psC[nb].t[:], diag[b].t[:, k, :], u.t[:, c, n * 512 + k:n * 512 + k + 512],
                                                      start=(k == 0), stop=(k == CW - 1)),
                             r=[diag[b].b, u.b], w=[psC[nb].b], signal=(k == CW - 1))
                    p.op('act', lambda e: e.activation(out=conv.t[:, c, n * 512:(n + 1) * 512], in_=psC[nb].t[:],
                                                       func=AF.Identity, bias=self.pc('dwb', c)),
                         r=[psC[nb].b, self.pcol.b], w=[conv.b])
            p.barrier()
            lw.close()
            self.dump('u', u, [128, 8, T + 30], BF16)
            self.dump('conv', conv, [128, 8, T], F32)
            with ExitStack() as l2:
                sq = [self.sb(l2, "sq%d" % i, [128, 512], F32) for i in range(2)]
                mean = self.sb(l2, "mean", [128, 512], F32)
                msq = self.sb(l2, "msq", [128, 512], F32)
                var = self.sb(l2, "var", [128, 512], F32)
                rs = self.sb(l2, "rs", [128, 512], F32)
                tmp = [self.sb(l2, "tmpc%d" % i, [128, 512], F32) for i in range(2)]
                for n in range(4):
                    ns = slice(n * 512, (n + 1) * 512)
                    for c in range(8):
                        p.op('pe', lambda e: e.matmul(S1.t[:], self.ones_f.t[:], conv.t[:, c, ns], start=(c == 0), stop=(c == 7)),
                             r=[self.ones_f.b, conv.b], w=[S1.b], signal=(c == 7))
                    for c in range(8):
                        sb_ = c % 2
                        p.op('act', lambda e: e.activation(out=sq[sb_].t[:], in_=conv.t[:, c, ns], func=AF.Square),
                             r=[conv.b], w=[sq[sb_].b])
                        p.op('pe', lambda e: e.matmul(S2.t[:], self.ones_f.t[:], sq[sb_].t[:], start=(c == 0), stop=(c == 7)),
                             r=[self.ones_f.b, sq[sb_].b], w=[S2.b], signal=True)
                    p.op('act', lambda e: e.activation(out=msq.t[:], in_=S1.t[:], func=AF.Square, scale=1.0 / CONV_CH),
                         r=[S1.b], w=[msq.b])
                    p.op('act', lambda e: e.activation(out=mean.t[:], in_=S1.t[:], func=AF.Copy, scale=1.0 / CONV_CH),
                         r=[S1.b], w=[mean.b])
                    p.op('dve', lambda e: e.scalar_tensor_tensor(out=var.t[:], in0=S2.t[:], scalar=1.0 / CONV_CH, in1=msq.t[:],
                                                                 op0=ALU.mult, op1=ALU.subtract),
                         r=[S2.b, msq.b], w=[var.b])
                    p.op('act', lambda e: e.activation(out=var.t[:], in_=var.t[:], func=AF.Sqrt, bias=self.epsc(), scale=1.0),
                         r=[var.b, self.cst.b], w=[var.b])
                    p.op('dve', lambda e: e.reciprocal(out=rs.t[:], in_=var.t[:]), r=[var.b], w=[rs.b])
                    for c in range(8):
                        tb_ = c % 2
                        p.op('dve', lambda e: e.tensor_tensor(out=tmp[tb_].t[:], in0=conv.t[:, c, ns], in1=mean.t[:], op=ALU.subtract),
                             r=[conv.b, mean.b], w=[tmp[tb_].b])
                        p.op('dve', lambda e: e.tensor_tensor(out=tmp[tb_].t[:], in0=tmp[tb_].t[:], in1=rs.t[:], op=ALU.mult),
                             r=[tmp[tb_].b, rs.b], w=[tmp[tb_].b])
                        p.op('act', lambda e: e.activation(out=u.t[:, c, 30 + n * 512:30 + (n + 1) * 512], in_=tmp[tb_].t[:],
                                                           func=AF.Silu, bias=self.pc('lnb', c), scale=self.pc('lng', c)),
                             r=[tmp[tb_].b, self.pcol.b], w=[u.b])
            p.barrier()
        self.dump('aT', u, [128, 8, T + 30], BF16)

    def qk_chunk(self, w, dst_ap, dstb, gcol, gcolb, psQ, psS, sqb, rr):
        for n in range(4):
            self.qk_part(w, dst_ap, dstb, gcol, gcolb, psQ, psS, sqb, rr, n)

    def qk_part(self, w, dst_ap, dstb, gcol, gcolb, psQ, psS, sqb, rr, n):
        p = self.p
        xnT = self.xnT
        if True:
            nb = n % 2
            ns = slice(n * 512, (n + 1) * 512)
            for kc in range(KC):
                p.op('pe', lambda e: e.matmul(psQ[nb].t[:], w.t[:, kc, :], xnT.t[:, kc, ns], start=(kc == 0), stop=(kc == KC - 1)),
                     r=[w.b, xnT.b], w=[psQ[nb].b], signal=(kc == KC - 1))
            p.op('act', lambda e: e.activation(out=sqb[nb].t[:], in_=psQ[nb].t[:], func=AF.Square), r=[psQ[nb].b], w=[sqb[nb].b])
            p.op('pe', lambda e: e.matmul(psS.t[:], self.blk_bf.t[:], sqb[nb].t[:], start=True, stop=True),
                 r=[self.blk_bf.b, sqb[nb].b], w=[psS.b])
            p.op('act', lambda e: e.activation(out=rr[nb].t[:], in_=psS.t[:], func=AF.Ln, bias=self.epsc(), scale=1.0 / HD),
                 r=[psS.b, self.cst.b], w=[rr[nb].b])
            p.op('act', lambda e: e.activation(out=rr[nb].t[:], in_=rr[nb].t[:], func=AF.Exp, scale=-0.5), r=[rr[nb].b], w=[rr[nb].b])
            p.op('dve', lambda e: e.scalar_tensor_tensor(out=dst_ap[:, ns], in0=psQ[nb].t[:], scalar=gcol, in1=rr[nb].t[:],
                                                         op0=ALU.mult, op1=ALU.mult),
                 r=[psQ[nb].b, rr[nb].b, gcolb], w=[dstb])

    def phaseC(self):
        nc, p, I = self.nc, self.p, self.I
        xnT, aT, oT = self.xnT, self.u, self.oT
        self.mergedS = nc.dram_tensor("mergedS", [128, KC, T], BF16, kind="Internal").ap()
        self.mergedSb = p.buf("mergedS")
        with ExitStack() as les:
            wco = [self.sb(les, "wco%d" % i, [128, 8, 128], BF16) for i in range(2)]
            wao = [self.sb(les, "wao%d" % i, [128, 8, 128], BF16) for i in range(2)]
            wg0 = [self.sb(les, "wg0%d" % i, [128, KC, 128], BF16) for i in range(2)]
            wg1 = [self.sb(les, "wg1%d" % i, [128, KC, 128], BF16) for i in range(2)]
            s0 = [self.sb(les, "s0%d" % i, [128, 512], F32) for i in range(2)]
            s1 = [self.sb(les, "s1%d" % i, [128, 512], F32) for i in range(2)]
            mg = [self.sb(les, "mg%d" % i, [128, T], BF16) for i in range(2)]
            psYc = [self.ps(les, "psYc%d" % i, [128, 512]) for i in range(2)]
            psYa = [self.ps(les, "psYa%d" % i, [128, 512]) for i in range(2)]
            psG0 = [self.ps(les, "psG0%d" % i, [128, 512]) for i in range(2)]
            psG1 = [self.ps(les, "psG1%d" % i, [128, 512]) for i in range(2)]
            def loadC(j):
                b = j % 2
                cs = slice(j * 128, (j + 1) * 128)
                self.load_w(wg0[b].t[:], wg0[b].b, I['w_in'][:, OFF_G + j * 128:OFF_G + (j + 1) * 128])
                self.load_w(wg1[b].t[:], wg1[b].b, I['w_in'][:, OFF_G + D + j * 128:OFF_G + D + (j + 1) * 128])
                self.load_w(wco[b].t[:], wco[b].b, I['w_conv_out'][:, cs])
                self.load_w(wao[b].t[:], wao[b].b, I['w_attn_out'][:, cs])
                if j % 3 == 1 or j == KC - 1:
                    self.precast_next(after=psYa[1].b)
            self.zero_fill_next(100)
            loadC(0)
            for j in range(KC):
                b = j % 2
                if j + 1 < KC:
                    loadC(j + 1)
                for n in range(4):
                    nb = n % 2
                    ns = slice(n * 512, (n + 1) * 512)
                    for kc in range(KC):
                        p.op('pe', lambda e: e.matmul(psG0[nb].t[:], wg0[b].t[:, kc, :], xnT.t[:, kc, ns], start=(kc == 0), stop=(kc == KC - 1)),
                             r=[wg0[b].b, xnT.b], w=[psG0[nb].b], signal=(kc == KC - 1))
                    for kc in range(KC):
                        p.op('pe', lambda e: e.matmul(psG1[nb].t[:], wg1[b].t[:, kc, :], xnT.t[:, kc, ns], start=(kc == 0), stop=(kc == KC - 1)),
                             r=[wg1[b].b, xnT.b], w=[psG1[nb].b], signal=(kc == KC - 1))
                    for kc in range(8):
                        p.op('pe', lambda e: e.matmul(psYc[nb].t[:], wco[b].t[:, kc, :], aT.t[:, kc, 30 + n * 512:30 + (n + 1) * 512],
                                                      start=(kc == 0), stop=(kc == 7)),
                             r=[wco[b].b, aT.b], w=[psYc[nb].b], signal=(kc == 7))
                    for kc in range(8):
                        p.op('pe', lambda e: e.matmul(psYa[nb].t[:], wao[b].t[:, kc, :], oT.t[:, kc, ns], start=(kc == 0), stop=(kc == 7)),
                             r=[wao[b].b, oT.b], w=[psYa[nb].b], signal=(kc == 7))
                    p.op('act', lambda e: e.activation(out=s0[nb].t[:], in_=psG0[nb].t[:], func=AF.Sigmoid), r=[psG0[nb].b], w=[s0[nb].b])
                    p.op('act', lambda e: e.activation(out=s1[nb].t[:], in_=psG1[nb].t[:], func=AF.Sigmoid), r=[psG1[nb].b], w=[s1[nb].b])
                    p.op('dve', lambda e: e.tensor_tensor(out=s0[nb].t[:], in0=psYc[nb].t[:], in1=s0[nb].t[:], op=ALU.mult),
                         r=[psYc[nb].b, s0[nb].b], w=[s0[nb].b])
                    p.op('dve', lambda e: e.tensor_tensor(out=s1[nb].t[:], in0=psYa[nb].t[:], in1=s1[nb].t[:], op=ALU.mult),
                         r=[psYa[nb].b, s1[nb].b], w=[s1[nb].b])
                    p.op('pool', lambda e: e.tensor_tensor(out=mg[b].t[:, ns], in0=s0[nb].t[:], in1=s1[nb].t[:], op=ALU.add),
                         r=[s0[nb].b, s1[nb].b], w=[mg[b].b])
                p.dma('sp', [(self.mergedS[:, j, :], mg[b].t[:])], r=[mg[b].b], w=[self.mergedSb])
            p.barrier()
        if 'merged' in self.debug:
            o = nc.dram_tensor("dbg_merged", [128, KC, T], BF16, kind="ExternalOutput").ap()
            ob = p.buf("dbg_merged")
            p.dma('sp', [(o, self.mergedS)], r=[self.mergedSb], w=[ob])
            self.dbg_out['merged'] = ob

    def phaseM(self):
        nc, p, I = self.nc, self.p, self.I
        kxT, vx = self.kxT, self.vx
        with ExitStack() as l2:
            memnT = self.sb(l2, "memnT", [128, KC, MEM], BF16)
            mt = self.sb(l2, "mt", [128, D], F32)
            ms = self.sb(l2, "ms", [128, D], BF16)
            junk = self.sb(l2, "junkM", [128, D], BF16)
            ssq = self.sb(l2, "ssqM", [128, 1], F32)
            sd = self.sb(l2, "sdM", [128, 1], F32)
            rstd = self.sb(l2, "rstdM", [128, 1], F32)
            wkv = self.sb(l2, "wkv", [128, KC, XW], BF16)
            sqk = self.sb(l2, "sqk", [128, MEM], BF16)
            rk = self.sb(l2, "rk", [128, MEM], F32)
            xkgs = self.sb(l2, "xkgs", [128, 1], F32)
            pTm = self.ps(l2, "pTm", [128, D], BF16)
            psK = self.ps(l2, "psK", [128, 512])
            psK2 = self.ps(l2, "psK2", [128, 512])
            gmem = self.pcol.t[:, PCOL['gmem']:PCOL['gmem'] + KC]
            for mtile in range(2):
                p.dma('sp', [(mt.t[:], I['mem'][mtile * 128:(mtile + 1) * 128, :])], w=[mt.b])
                self.rms_stats(mt.t[:], mt.b, junk, ssq, sd, rstd)
                p.op('act', lambda e: e.activation(out=ms.t[:], in_=mt.t[:], func=AF.Copy, scale=rstd.t[:]),
                     r=[mt.b, rstd.b], w=[ms.b])
                for kc in range(KC):
                    p.op('pe', lambda e: e.transpose(pTm.t[:, kc * 128:(kc + 1) * 128], ms.t[:, kc * 128:(kc + 1) * 128], self.ident_bf.t[:]),
                         r=[ms.b, self.ident_bf.b], w=[pTm.b], signal=(kc == KC - 1))
                p.op('dve', lambda e: e.tensor_tensor(out=memnT.t[:, :, mtile * 128:(mtile + 1) * 128],
                                                      in0=pTm.t[:].rearrange("p (k t) -> p k t", k=KC),
                                                      in1=gmem.unsqueeze(2).to_broadcast([128, KC, 128]), op=ALU.mult),
                     r=[pTm.b, self.pcol.b], w=[memnT.b])
            p.op('dve', lambda e: e.tensor_scalar(out=xkgs.t[:], in0=self.pc('xkg'), scalar1=XHD ** -0.5, scalar2=None, op0=ALU.mult),
                 r=[self.pcol.b], w=[xkgs.b])
            self.load_w(wkv.t[:], wkv.b, I['w_xkv'][:, 0:XW])
            for hh in range(XH):
                for kc in range(KC):
                    p.op('pe', lambda e: e.matmul(psK.t[:, 0:MEM], wkv.t[:, kc, hh * 128:(hh + 1) * 128], memnT.t[:, kc, :],
                                                  start=(kc == 0), stop=(kc == KC - 1)),
                         r=[wkv.b, memnT.b], w=[psK.b], signal=(kc == KC - 1))
                p.op('act', lambda e: e.activation(out=sqk.t[:], in_=psK.t[:, 0:MEM], func=AF.Square), r=[psK.b], w=[sqk.b])
                p.op('pe', lambda e: e.matmul(psK2.t[:, 0:MEM], self.ones_bf.t[:], sqk.t[:], start=True, stop=True),
                     r=[self.ones_bf.b, sqk.b], w=[psK2.b])
                p.op('act', lambda e: e.activation(out=rk.t[:], in_=psK2.t[:, 0:MEM], func=AF.Sqrt, bias=self.epsc(), scale=1.0 / XHD),
                     r=[psK2.b, self.cst.b], w=[rk.b])
                p.op('dve', lambda e: e.reciprocal(out=rk.t[:], in_=rk.t[:]), r=[rk.b], w=[rk.b])
                p.op('dve', lambda e: e.scalar_tensor_tensor(out=kxT.t[:, hh, :], in0=psK.t[:, 0:MEM], scalar=xkgs.t[:], in1=rk.t[:],
                                                             op0=ALU.mult, op1=ALU.mult),
                     r=[psK.b, xkgs.b, rk.b], w=[kxT.b])
            self.load_w(wkv.t[:], wkv.b, I['w_xkv'][:, XW:2 * XW])
            for mc in range(2):
                for kc in range(KC):
                    p.op('pe', lambda e: e.matmul(psK.t[:], memnT.t[:, kc, mc * 128:(mc + 1) * 128], wkv.t[:, kc, :],
                                                  start=(kc == 0), stop=(kc == KC - 1)),
                         r=[wkv.b, memnT.b], w=[psK.b], signal=(kc == KC - 1))
                p.op('act', lambda e: e.activation(out=vx.t[:, mc, :], in_=psK.t[:], func=AF.Copy), r=[psK.b], w=[vx.b])
            p.barrier()

    def phaseD(self):
        nc, p, I = self.nc, self.p, self.I
        es = self.es
        self.h2S = nc.dram_tensor("h2S", [T, D], F32, kind="Internal").ap()
        self.h2Sb = p.buf("h2S")
        self.logits = self.sb(es, "logits", [128, NT, 36], F32)
        self.rstd2 = self.sb(es, "rstd2", [128, NT], F32)
        logits, rstd2 = self.logits, self.rstd2
        with ExitStack() as les:
            kxT, vx = self.kxT, self.vx
            wmix = self.sb(les, "wmix", [128, KC, D], BF16)
            wxq = self.sb(les, "wxq", [128, KC, XW], BF16)
            wxo = self.sb(les, "wxo", [128, XH, D], BF16)
            wrt = self.sb(les, "wrt", [128, KC, 36], F32)
            wmixb = [p.buf("wmix_cg%d" % cg) for cg in range(4)]
            for cg in range(4):
                p.dma('pool', [(wmix.t[:, :, cg * 512:(cg + 1) * 512],
                                I['w_mix_out'][:, cg * 512:(cg + 1) * 512].rearrange("(k p) c -> p k c", p=128))], w=[wmixb[cg]])
            self.load_w(wxq.t[:], wxq.b, I['w_xq'])
            for cg in range(4):
                p.dma('pool', [(wxo.t[:, :, cg * 512:(cg + 1) * 512],
                                I['w_xo'][:, cg * 512:(cg + 1) * 512].rearrange("(k p) c -> p k c", p=128))], w=[wxo.b])
            p.dma('sp', [(wrt.t[:], I['w_rt'].rearrange("(k p) c -> p k c", p=128))], w=[wrt.b])
            gx = self.pcol.t[:, PCOL['gx']:PCOL['gx'] + KC]
            gmoe = self.pcol.t[:, PCOL['gmoe']:PCOL['gmoe'] + KC]
            xqg = self.prow.t[:, PROW['xqg']:PROW['xqg'] + XW]
            rtb = self.prow.t[:, PROW['rtb']:PROW['rtb'] + 36]
            idf = self.cst.t[:, CST['ident']:CST['ident'] + 128]

            class Lane:
                pass
            lanes = []
            for li in range(2):
                L = Lane()
                n = lambda s_: "%s_%d" % (s_, li)
                L.mgt = self.sb(les, n("mgt"), [128, KC, 128], BF16)
                L.xt = self.sb(les, n("xtD"), [128, D], F32)
                L.h = self.sb(les, n("hD"), [128, D], F32)
                L.xs = self.sb(les, n("xsD"), [128, D], BF16)
                L.hn1T = self.sb(les, n("hn1T"), [128, KC, 128], BF16)
                L.hn2T = self.sb(les, n("hn2T"), [128, KC, 128], F32)
                L.ssq = self.sb(les, n("ssqD"), [128, 1], F32)
                L.sd = self.sb(les, n("sdD"), [128, 1], F32)
                L.rstd = self.sb(les, n("rstdD"), [128, 1], F32)
                L.sq = self.sb(les, n("sqD"), [128, XW], F32)
                L.ssq4 = self.sb(les, n("ssq4"), [128, XH], F32)
                L.r4 = self.sb(les, n("r4"), [128, XH], F32)
                L.qf = self.sb(les, n("qf"), [128, XW], F32)
                L.qn = self.sb(les, n("qn"), [128, XW], BF16)
                L.qxT = self.sb(les, n("qxT"), [128, XH, 128], BF16)
                L.pxT = self.sb(les, n("pxT"), [128, XH, 2, 128], BF16)
                L.rdx = self.sb(les, n("rdx"), [128, XW], F32)
                L.oxT = self.sb(les, n("oxT"), [128, XH, 128], BF16)
                L.PA = self.ps(les, n("PA"), [128, D])
                L.B01 = L.PA.b
                L.B2 = p.buf(n("PA_b2"))
                L.B3 = p.buf(n("PA_b3"))
                L.PAll = [L.B01, L.B2, L.B3]
                L.P01 = L.PA.t[:, 0:1024]
                L.P01b = L.PA.t[:, 0:1024].bitcast(BF16)
                L.P2 = L.PA.t[:, 1024:1536]
                L.P3 = L.PA.t[:, 1536:2048]
                L.P3b = L.PA.t[:, 1536:2048].bitcast(BF16)
                lanes.append(L)

            def s_load(L, tt):
                ts_ = slice(tt * 128, (tt + 1) * 128)
                p.dma('sp', [(L.mgt.t[:], self.mergedS[:, :, ts_])], r=[self.mergedSb], w=[L.mgt.b])
                p.dma('sp', [(L.xt.t[:], I['x'][ts_, :])], w=[L.xt.b])

            def s_mix(L, tt):
                for cg in range(4):
                    for kc in range(KC):
                        p.op('pe', lambda e: e.matmul(L.PA.t[:, cg * 512:(cg + 1) * 512], L.mgt.t[:, kc, :], wmix.t[:, kc, cg * 512:(cg + 1) * 512],
                                                      start=(kc == 0), stop=(kc == KC - 1)),
                             r=[L.mgt.b, wmixb[cg]], w=L.PAll, signal=(cg == 3 and kc == KC - 1))
                p.op('dve', lambda e: e.tensor_tensor(out=L.h.t[:], in0=L.PA.t[:], in1=L.xt.t[:], op=ALU.add), r=L.PAll + [L.xt.b], w=[L.h.b])

            def stats(L):
                p.op('act', lambda e: e.activation(out=L.xs.t[:], in_=L.h.t[:], func=AF.Square, accum_out=L.ssq.t[:]),
                     r=[L.h.b], w=[L.xs.b, L.ssq.b])
                p.op('act', lambda e: e.activation(out=L.sd.t[:], in_=L.ssq.t[:], func=AF.Sqrt, bias=self.epsc(), scale=1.0 / D),
                     r=[L.ssq.b, self.cst.b], w=[L.sd.b])
                p.op('dve', lambda e: e.reciprocal(out=L.rstd.t[:], in_=L.sd.t[:]), r=[L.sd.b], w=[L.rstd.b])

            def s_norm1(L, tt):
                stats(L)
                p.op('act', lambda e: e.activation(out=L.xs.t[:], in_=L.h.t[:], func=AF.Copy, scale=L.rstd.t[:]), r=[L.h.b, L.rstd.b], w=[L.xs.b])

            def s_tr1(L, tt):
                for kc in range(KC):
                    p.op('pe', lambda e: e.transpose(L.P01b[:, kc * 128:(kc + 1) * 128], L.xs.t[:, kc * 128:(kc + 1) * 128], self.ident_bf.t[:]),
                         r=[L.xs.b, self.ident_bf.b], w=[L.B01], signal=(kc == KC - 1))
                p.op('dve', lambda e: e.tensor_tensor(out=L.hn1T.t[:], in0=L.P01b.rearrange("p (k t) -> p k t", k=KC),
                                                      in1=gx.unsqueeze(2).to_broadcast([128, KC, 128]), op=ALU.mult),
                     r=[L.B01, self.pcol.b], w=[L.hn1T.b])

            def s_qa(L, tt):
                for kc in range(KC):
                    p.op('pe', lambda e: e.matmul(L.P2, L.hn1T.t[:, kc, :], wxq.t[:, kc, :], start=(kc == 0), stop=(kc == KC - 1)),
                         r=[L.hn1T.b, wxq.b], w=[L.B2], signal=(kc == KC - 1))
                p.op('act', lambda e: e.activation(out=L.sq.t[:], in_=L.P2, func=AF.Square), r=[L.B2], w=[L.sq.b])
                p.op('dve', lambda e: e.tensor_reduce(out=L.ssq4.t[:], in_=L.sq.t[:].rearrange("p (h d) -> p h d", h=XH), axis=AX.X, op=ALU.add),
                     r=[L.sq.b], w=[L.ssq4.b])
                p.op('act', lambda e: e.activation(out=L.r4.t[:], in_=L.ssq4.t[:], func=AF.Sqrt, bias=self.epsc(), scale=1.0 / XHD),
                     r=[L.ssq4.b, self.cst.b], w=[L.r4.b])
                p.op('dve', lambda e: e.reciprocal(out=L.r4.t[:], in_=L.r4.t[:]), r=[L.r4.b], w=[L.r4.b])
                p.op('dve', lambda e: e.tensor_tensor(out=L.qf.t[:].rearrange("p (h d) -> p h d", h=XH),
                                                      in0=L.P2.rearrange("p (h d) -> p h d", h=XH),
                                                      in1=L.r4.t[:].unsqueeze(2).to_broadcast([128, XH, XHD]), op=ALU.mult),
                     r=[L.B2, L.r4.b], w=[L.qf.b])
                p.op('dve', lambda e: e.tensor_tensor(out=L.qn.t[:], in0=L.qf.t[:], in1=xqg, op=ALU.mult), r=[L.qf.b, self.prow.b], w=[L.qn.b])

            def s_qb(L, tt):
                for hh in range(XH):
                    p.op('pe', lambda e: e.transpose(L.P3b[:, hh * 128:(hh + 1) * 128], L.qn.t[:, hh * 128:(hh + 1) * 128], self.ident_bf.t[:]),
                         r=[L.qn.b, self.ident_bf.b], w=[L.B3], signal=(hh == XH - 1))
                p.op('act', lambda e: e.activation(out=L.qxT.t[:], in_=L.P3b[:, 0:XW].rearrange("p (h t) -> p h t", h=XH), func=AF.Copy),
                     r=[L.B3], w=[L.qxT.b])

            def s_at_a(L, tt):
                for hh in range(XH):
                    for mc in range(2):
                        o = (hh * 2 + mc) * 128
                        p.op('pe', lambda e: e.matmul(L.P01[:, o:o + 128], kxT.t[:, hh, mc * 128:(mc + 1) * 128], L.qxT.t[:, hh, :],
                                                      start=True, stop=True),
                             r=[kxT.b, L.qxT.b], w=[L.B01], signal=(hh == XH - 1 and mc == 1))
                p.op('act', lambda e: e.activation(out=L.pxT.t[:].rearrange("p h m t -> p (h m t)"), in_=L.P01, func=AF.Exp),
                     r=[L.B01], w=[L.pxT.b])

            def s_at_b(L, tt):
                for (pso, psb, get_l) in ((L.P2, L.B2, lambda hh, mc: vx.t[:, mc, hh * 128:(hh + 1) * 128]),
                                          (L.P3, L.B3, lambda hh, mc: self.ones_bf.t[:])):
                    for hh in range(XH):
                        for mc in range(2):
                            p.op('pe', lambda e: e.matmul(pso[:, hh * 128:(hh + 1) * 128], get_l(hh, mc), L.pxT.t[:, hh, mc, :],
                                                          start=(mc == 0), stop=(mc == 1)),
                                 r=[vx.b, self.ones_bf.b, L.pxT.b], w=[psb], signal=(hh == XH - 1 and mc == 1))
                p.op('dve', lambda e: e.reciprocal(out=L.rdx.t[:], in_=L.P3), r=[L.B3], w=[L.rdx.b])
                p.op('dve', lambda e: e.tensor_tensor(out=L.oxT.t[:].rearrange("p h t -> p (h t)"), in0=L.P2, in1=L.rdx.t[:], op=ALU.mult),
                     r=[L.B2, L.rdx.b], w=[L.oxT.b])

            def s_xo(L, tt):
                ts_ = slice(tt * 128, (tt + 1) * 128)
                for cg in range(4):
                    for hh in range(XH):
                        p.op('pe', lambda e: e.matmul(L.PA.t[:, cg * 512:(cg + 1) * 512], L.oxT.t[:, hh, :], wxo.t[:, hh, cg * 512:(cg + 1) * 512],
                                                      start=(hh == 0), stop=(hh == XH - 1)),
                             r=[L.oxT.b, wxo.b], w=L.PAll, signal=(cg == 3 and hh == XH - 1))
                p.op('dve', lambda e: e.tensor_tensor(out=L.h.t[:], in0=L.PA.t[:], in1=L.h.t[:], op=ALU.add), r=L.PAll + [L.h.b], w=[L.h.b])
                p.dma('sp', [(self.h2S[ts_, :], L.h.t[:])], r=[L.h.b], w=[self.h2Sb])
                stats(L)
                p.op('dve', lambda e: e.tensor_copy(out=rstd2.t[:, tt:tt + 1], in_=L.rstd.t[:]), r=[L.rstd.b], w=[rstd2.b])
                p.op('act', lambda e: e.activation(out=L.h.t[:], in_=L.h.t[:], func=AF.Copy, scale=L.rstd.t[:]), r=[L.h.b, L.rstd.b], w=[L.h.b])

            def s_tr2(L, tt):
                for kc in range(KC):
                    p.op('pe', lambda e: e.transpose(L.PA.t[:, kc * 128:(kc + 1) * 128], L.h.t[:, kc * 128:(kc + 1) * 128], idf),
                         r=[L.h.b, self.cst.b], w=L.PAll, signal=(kc == KC - 1))
                p.op('dve', lambda e: e.tensor_tensor(out=L.hn2T.t[:], in0=L.PA.t[:].rearrange("p (k t) -> p k t", k=KC),
                                                      in1=gmoe.unsqueeze(2).to_broadcast([128, KC, 128]), op=ALU.mult),
                     r=L.PAll + [self.pcol.b], w=[L.hn2T.b])

            def s_rt(L, tt):
                for kc in range(KC):
                    p.op('pe', lambda e: e.matmul(L.P2[:, 0:36], L.hn2T.t[:, kc, :], wrt.t[:, kc, :], start=(kc == 0), stop=(kc == KC - 1)),
                         r=[L.hn2T.b, wrt.b], w=[L.B2], signal=(kc == KC - 1))
                p.op('dve', lambda e: e.tensor_tensor(out=logits.t[:, tt, :], in0=L.P2[:, 0:36], in1=rtb, op=ALU.add),
                     r=[L.B2, self.prow.b], w=[logits.b])

            stages = [s_mix, s_norm1, s_tr1, s_qa, s_qb, s_at_a, s_at_b, s_xo, s_tr2, s_rt]
            s_load(lanes[0], 0)
            s_load(lanes[1], 1)
            for pi in range(NT // 2):
                t0, t1 = 2 * pi, 2 * pi + 1
                for st in stages:
                    st(lanes[0], t0)
                    st(lanes[1], t1)
                    if st is s_mix:
                        if pi + 1 < NT // 2:
                            s_load(lanes[0], t0 + 2)
                            s_load(lanes[1], t1 + 2)
            p.barrier()
        self.dump('logits', logits, [128, NT, 36], F32)
        if 'h2' in self.debug:
            o = nc.dram_tensor("dbg_h2", [T, D], F32, kind="ExternalOutput").ap()
            ob = p.buf("dbg_h2")
            p.dma('sp', [(o, self.h2S)], r=[self.h2Sb], w=[ob])
            self.dbg_out['h2'] = ob

    def phaseR(self):
        nc, p, I = self.nc, self.p, self.I
        es = self.es
        self.precast_next(NE)
        L = self.logits
        NR = NE * CAP
        self.ysS = nc.dram_tensor("ysS", [NR + 1, D], BF16, kind="Internal").ap()
        self.xs_t = [p.buf("xs_t%d" % i) for i in range(4)]
        self.ys_zero = p.buf("ys_zero")
        self.gate = [self.sb(es, "gate%d" % k, [128, NT], F32) for k in range(2)]
        self.dsti = [self.sb(es, "dsti%d" % k, [128, NT], I32) for k in range(2)]
        self.gmoe_row = self.sb(es, "gmoe_row", [128, D], F32)
        p.dma('sp', [(self.gmoe_row.t[:], I['gmoe_row'])], w=[self.gmoe_row.b])
        with ExitStack() as les:
            zt = self.sb(les, "zt", [128, D], F32)
            p.op('dve', lambda e: e.memset(zt.t[:], 0.0), w=[zt.b])
            ztb = zt.t[:].bitcast(BF16)
            p.dma('sp', [(self.ysS[NR:NR + 1, :], ztb[0:1, 0:D])], r=[zt.b], w=[self.ys_zero])

            def t2(name, n, dt=F32):
                return self.sb(les, name, [128, NT, n] if n else [128, NT], dt)
            lg = L.t[:, :, 0:NG]
            le4 = L.t[:, :, NG:NG + NE].rearrange("p t (g e) -> p t g e", g=NG)
            gmax = t2("gmax", 0)
            ohg = t2("ohg", NG)
            lgs = t2("lgs", NG)
            sum4 = t2("sum4", 0)
            pg = t2("pg", 0)
            tmp48 = t2("tmp48", NE)
            lesel = t2("lesel", EPG)
            lesel2 = t2("lesel2", EPG)
            m1 = t2("m1", 0)
            m2 = t2("m2", 0)
            oh = [t2("oh1", EPG), t2("oh2", EPG)]
            dm = t2("dm", 0)
            w1 = t2("w1", 0)
            Mk = [t2("Mk1", NE), t2("Mk2", NE)]
            Mb = t2("Mb", NE, BF16)
            tot = t2("tot", NE)
            carry = t2("carry", NE)
            pos = t2("pos", NE)
            pose = t2("pose", NE)
            ecap = self.sb(les, "ecap", [128, NE], F32)
            posk = t2("posk", 0)
            dk = t2("dk", 0)
            valid = t2("valid", 0)
            tri_bf = self.sb(les, "tri_bf", [128, 128], BF16)
            psP = self.ps(les, "psP", [128, 512])
            psTot = self.ps(les, "psTot", [128, 512])
            V = 'dve'

            def bc(ap2, n):
                return ap2.unsqueeze(2).to_broadcast([128, NT, n])
            p.op(V, lambda e: e.tensor_copy(out=tri_bf.t[:], in_=self.cst.t[:, CST['tri']:CST['tri'] + 128]), r=[self.cst.b], w=[tri_bf.b])
            p.op(V, lambda e: e.tensor_reduce(out=gmax.t[:], in_=lg, axis=AX.X, op=ALU.max), r=[L.b], w=[gmax.b])
            p.op(V, lambda e: e.tensor_tensor(out=ohg.t[:], in0=lg, in1=bc(gmax.t[:], NG), op=ALU.is_equal), r=[L.b, gmax.b], w=[ohg.b])
            p.op(V, lambda e: e.tensor_tensor(out=lgs.t[:], in0=lg, in1=bc(gmax.t[:], NG), op=ALU.subtract), r=[L.b, gmax.b], w=[lgs.b])
            p.op('act', lambda e: e.activation(out=lgs.t[:], in_=lgs.t[:], func=AF.Exp), r=[lgs.b], w=[lgs.b])
            p.op(V, lambda e: e.tensor_reduce(out=sum4.t[:], in_=lgs.t[:], axis=AX.X, op=ALU.add), r=[lgs.b], w=[sum4.b])
            p.op(V, lambda e: e.reciprocal(out=pg.t[:], in_=sum4.t[:]), r=[sum4.b], w=[pg.b])
            t48 = tmp48.t[:].rearrange("p t (g e) -> p t g e", g=NG)
            p.op(V, lambda e: e.tensor_tensor(out=t48, in0=le4, in1=ohg.t[:].unsqueeze(3).to_broadcast([128, NT, NG, EPG]), op=ALU.mult),
                 r=[L.b, ohg.b], w=[tmp48.b])
            p.op(V, lambda e: e.tensor_reduce(out=lesel.t[:], in_=tmp48.t[:].rearrange("p t (g e) -> p t e g", g=NG), axis=AX.X, op=ALU.add),
                 r=[tmp48.b], w=[lesel.b])
            p.op(V, lambda e: e.tensor_reduce(out=m1.t[:], in_=lesel.t[:], axis=AX.X, op=ALU.max), r=[lesel.b], w=[m1.b])
            p.op(V, lambda e: e.tensor_tensor(out=oh[0].t[:], in0=lesel.t[:], in1=bc(m1.t[:], EPG), op=ALU.is_equal),
                 r=[lesel.b, m1.b], w=[oh[0].b])
            p.op(V, lambda e: e.scalar_tensor_tensor(out=lesel2.t[:], in0=oh[0].t[:], scalar=-1e30, in1=lesel.t[:], op0=ALU.mult, op1=ALU.add),
                 r=[oh[0].b, lesel.b], w=[lesel2.b])
            p.op(V, lambda e: e.tensor_reduce(out=m2.t[:], in_=lesel2.t[:], axis=AX.X, op=ALU.max), r=[lesel2.b], w=[m2.b])
            p.op(V, lambda e: e.tensor_tensor(out=oh[1].t[:], in0=lesel2.t[:], in1=bc(m2.t[:], EPG), op=ALU.is_equal),
                 r=[lesel2.b, m2.b], w=[oh[1].b])
            p.op(V, lambda e: e.tensor_tensor(out=dm.t[:], in0=m2.t[:], in1=m1.t[:], op=ALU.subtract), r=[m1.b, m2.b], w=[dm.b])
            p.op('act', lambda e: e.activation(out=dm.t[:], in_=dm.t[:], func=AF.Exp), r=[dm.b], w=[dm.b])
            p.op(V, lambda e: e.tensor_scalar(out=w1.t[:], in0=dm.t[:], scalar1=1.0, scalar2=None, op0=ALU.add), r=[dm.b], w=[w1.b])
            p.op(V, lambda e: e.reciprocal(out=w1.t[:], in_=w1.t[:]), r=[w1.b], w=[w1.b])
            g1, g2 = self.gate
            p.op(V, lambda e: e.tensor_tensor(out=g1.t[:], in0=pg.t[:], in1=w1.t[:], op=ALU.mult), r=[pg.b, w1.b], w=[g1.b])
            p.op(V, lambda e: e.tensor_tensor(out=g2.t[:], in0=g1.t[:], in1=dm.t[:], op=ALU.mult), r=[g1.b, dm.b], w=[g2.b])
            for k in range(2):
                mk4 = Mk[k].t[:].rearrange("p t (g e) -> p t g e", g=NG)
                p.op(V, lambda e: e.tensor_tensor(out=mk4, in0=ohg.t[:].unsqueeze(3).to_broadcast([128, NT, NG, EPG]),
                                                  in1=oh[k].t[:].unsqueeze(2).to_broadcast([128, NT, NG, EPG]), op=ALU.mult),
                     r=[ohg.b, oh[k].b], w=[Mk[k].b])
            p.op(V, lambda e: e.tensor_tensor(out=Mb.t[:], in0=Mk[0].t[:], in1=Mk[1].t[:], op=ALU.add), r=[Mk[0].b, Mk[1].b], w=[Mb.b])
            Mbf = Mb.t[:].rearrange("p t e -> p (t e)")
            p.op('pe', lambda e: e.matmul(psP.t[:], tri_bf.t[:], Mbf, start=True, stop=True), r=[tri_bf.b, Mb.b], w=[psP.b])
            p.op('pe', lambda e: e.matmul(psTot.t[:], self.ones_bf.t[:], Mbf, start=True, stop=True), r=[self.ones_bf.b, Mb.b], w=[psTot.b])
            p.op(V, lambda e: e.tensor_copy(out=tot.t[:].rearrange("p t e -> p (t e)"), in_=psTot.t[:]), r=[psTot.b], w=[tot.b])
            p.op(V, lambda e: e.memset(carry.t[:, 0, :], 0.0), w=[carry.b])
            for tt in range(1, NT):
                p.op(V, lambda e: e.tensor_tensor(out=carry.t[:, tt, :], in0=carry.t[:, tt - 1, :], in1=tot.t[:, tt - 1, :], op=ALU.add),
                     r=[tot.b, carry.b], w=[carry.b])
            p.op(V, lambda e: e.tensor_tensor(out=pos.t[:].rearrange("p t e -> p (t e)"), in0=psP.t[:],
                                              in1=carry.t[:].rearrange("p t e -> p (t e)"), op=ALU.add), r=[psP.b, carry.b], w=[pos.b])
            p.op(V, lambda e: e.tensor_scalar(out=ecap.t[:], in0=self.cst.t[:, CST['iota32']:CST['iota32'] + NE], scalar1=float(CAP),
                                              scalar2=None, op0=ALU.mult), r=[self.cst.b], w=[ecap.b])
            p.op(V, lambda e: e.tensor_tensor(out=pose.t[:], in0=pos.t[:], in1=ecap.t[:].unsqueeze(1).to_broadcast([128, NT, NE]), op=ALU.add),
                 r=[pos.b, ecap.b], w=[pose.b])
            for k in range(2):
                p.op(V, lambda e: e.tensor_tensor(out=tmp48.t[:], in0=Mk[k].t[:], in1=pos.t[:], op=ALU.mult), r=[Mk[k].b, pos.b], w=[tmp48.b])
                p.op(V, lambda e: e.tensor_reduce(out=posk.t[:], in_=tmp48.t[:], axis=AX.X, op=ALU.add), r=[tmp48.b], w=[posk.b])
                p.op(V, lambda e: e.tensor_tensor(out=tmp48.t[:], in0=Mk[k].t[:], in1=pose.t[:], op=ALU.mult), r=[Mk[k].b, pose.b], w=[tmp48.b])
                p.op(V, lambda e: e.tensor_reduce(out=dk.t[:], in_=tmp48.t[:], axis=AX.X, op=ALU.add), r=[tmp48.b], w=[dk.b])
                p.op(V, lambda e: e.tensor_single_scalar(out=valid.t[:], in_=posk.t[:], scalar=float(CAP), op=ALU.is_lt), r=[posk.b], w=[valid.b])
                p.op(V, lambda e: e.tensor_tensor(out=self.gate[k].t[:], in0=self.gate[k].t[:], in1=valid.t[:], op=ALU.mult),
                     r=[self.gate[k].b, valid.b], w=[self.gate[k].b])
                p.op(V, lambda e: e.scalar_tensor_tensor(out=dk.t[:], in0=dk.t[:], scalar=-float(NR), in1=valid.t[:], op0=ALU.add, op1=ALU.mult),
                     r=[dk.b, valid.b], w=[dk.b])
                p.op(V, lambda e: e.tensor_scalar(out=dk.t[:], in0=dk.t[:], scalar1=float(NR), scalar2=None, op0=ALU.add), r=[dk.b], w=[dk.b])
                p.op(V, lambda e: e.tensor_copy(out=self.dsti[k].t[:], in_=dk.t[:]), r=[dk.b], w=[self.dsti[k].b])
            self.dump('gate0', self.gate[0], [128, NT], F32)
            self.dump('gate1', self.gate[1], [128, NT], F32)
            self.dump('dst0', self.dsti[0], [128, NT], I32)
            self.dump('dst1', self.dsti[1], [128, NT], I32)
            p.barrier()
        with ExitStack() as les:
            NBR = 4
            ht = [self.sb(les, "htR%d" % i, [128, D], F32) for i in range(NBR)]
            hb = [self.sb(les, "hbR%d" % i, [128, D], BF16) for i in range(NBR)]

            def loadR(tt):
                p.dma('sp', [(ht[tt % NBR].t[:], self.h2S[tt * 128:(tt + 1) * 128, :])], r=[self.h2Sb], w=[ht[tt % NBR].b])
            for tt in range(NBR - 1):
                loadR(tt)
            for tt in range(NT):
                b = tt % NBR
                if tt + NBR - 1 < NT:
                    loadR(tt + NBR - 1)
                p.op('dve', lambda e: e.scalar_tensor_tensor(out=hb[b].t[:], in0=ht[b].t[:], scalar=self.rstd2.t[:, tt:tt + 1],
                                                             in1=self.gmoe_row.t[:], op0=ALU.mult, op1=ALU.mult),
                     r=[ht[b].b, self.rstd2.b, self.gmoe_row.b], w=[hb[b].b])
                p.dma_custom('pool', lambda e, k: e.indirect_dma_start(
                    out=self.xsS[:, :], out_offset=bass.IndirectOffsetOnAxis(ap=self.dsti[k].t[:, tt:tt + 1], axis=0),
                    in_=hb[b].t[:, :], in_offset=None), 2,
                    r=[hb[b].b, self.dsti[0].b, self.dsti[1].b, self.xs_zero], w=[self.xs_t[tt % 4]])
            p.barrier()

        if 'xs' in self.debug:
            o = nc.dram_tensor("dbg_xs", [NR, D], BF16, kind="ExternalOutput").ap()
            ob = p.buf("dbg_xs")
            p.dma('sp', [(o, self.xsS[0:NR, :])], r=self.xs_t, w=[ob])
            self.dbg_out['xs'] = ob

    def phaseG(self):
        nc, p, I = self.nc, self.p, self.I
        self.ys_e = [p.buf("ys_e%d" % i) for i in range(4)]
        CTS = [(i * 128, min(128, CAP - i * 128)) for i in range((CAP + 127) // 128)]
        NCT = len(CTS)
        with ExitStack() as les:
            wgu = [self.sb(les, "wgu%d" % i, [128, KC, 2 * DE], BF16) for i in range(2)]
            wdn = [self.sb(les, "wdn%d" % i, [128, DE // 128, D], BF16) for i in range(2)]
            xe = [self.sb(les, "xe%d" % i, [128, NCT, D], BF16) for i in range(2)]
            XeT = [self.sb(les, "XeT%d" % i, [128, KC, CAP], BF16) for i in range(2)]
            actT = [self.sb(les, "actT%d" % i, [128, DE // 128, CAP], BF16) for i in range(2)]
            sg = [self.sb(les, "sg%d" % i, [128, CAP], F32) for i in range(2)]
            ye = [self.sb(les, "ye%d" % i, [128, D], BF16) for i in range(2)]
            pX = self.ps(les, "pX", [128, D], BF16)
            pG = [self.ps(les, "pG%d" % i, [128, 512]) for i in range(2)]
            pU = [self.ps(les, "pU%d" % i, [128, 512]) for i in range(2)]
            pY = [self.ps(les, "pY%d" % i, [128, 512]) for i in range(2)]
            yi = 0

            def loadG(ex):
                b = ex % 2
                if ex < NPRE:
                    rdep = [self.wB_g[ex // 8]]
                    p.dma('pool', [(wgu[b].t[:, hh * 8:(hh + 1) * 8, :].rearrange("p k c -> p (k c)"),
                                    self.wguB[ex * 128:(ex + 1) * 128, hh * 8 * 1024:(hh + 1) * 8 * 1024]) for hh in range(2)],
                          r=rdep, w=[wgu[b].b])
                    p.dma('pool', [(wdn[b].t[:].rearrange("p k c -> p (k c)"), self.wdnB[ex * 128:(ex + 1) * 128, :])],
                          r=rdep, w=[wdn[b].b])
                else:
                    p.dma('pool', [(wgu[b].t[:, q4 * 4:(q4 + 1) * 4, :],
                                    I['w_gu'][ex * D + q4 * 512:ex * D + (q4 + 1) * 512, :].rearrange("(k p) c -> p k c", p=128))
                                   for q4 in range(4)], w=[wgu[b].b])
                    p.dma('pool', [(wdn[b].t[:, :, cg * 1024:(cg + 1) * 1024],
                                    I['w_dn'][ex * DE:(ex + 1) * DE, cg * 1024:(cg + 1) * 1024].rearrange("(k p) c -> p k c", p=128))
                                   for cg in range(2)], w=[wdn[b].b])
                p.dma('sp', [(xe[b].t[0:rows, ct, :], self.xsS[ex * CAP + r0:ex * CAP + r0 + rows, :]) for ct, (r0, rows) in enumerate(CTS)],
                      r=[self.xs_zero] + self.xs_t, w=[xe[b].b])

            def transG(ex):
                b = ex % 2
                for ct, (r0, rows) in enumerate(CTS):
                    for kc in range(KC):
                        p.op('pe', lambda e: e.transpose(pX.t[:, kc * 128:kc * 128 + rows], xe[b].t[0:rows, ct, kc * 128:(kc + 1) * 128],
                                                         self.ident_bf.t[0:rows, 0:rows]),
                             r=[xe[b].b, self.ident_bf.b], w=[pX.b], signal=(kc == KC - 1))
                    if ct % 2 == 0:
                        p.op('act', lambda e: e.activation(out=XeT[b].t[:, :, r0:r0 + rows],
                                                           in_=pX.t[:].rearrange("p (k t) -> p k t", k=KC)[:, :, 0:rows], func=AF.Copy),
                             r=[pX.b], w=[XeT[b].b])
                    else:
                        p.op('dve', lambda e: e.tensor_copy(out=XeT[b].t[:, :, r0:r0 + rows],
                                                            in_=pX.t[:].rearrange("p (k t) -> p k t", k=KC)[:, :, 0:rows]),
                             r=[pX.b], w=[XeT[b].b])

            loadG(0)
            loadG(1)
            transG(0)
            for ex in range(NE):
                b = ex % 2
                for fc in range(DE // 128):
                    fb = fc % 2
                    for kc in range(KC):
                        p.op('pe', lambda e: e.matmul(pG[fb].t[:, 0:CAP], wgu[b].t[:, kc, fc * 128:(fc + 1) * 128], XeT[b].t[:, kc, :],
                                                      start=(kc == 0), stop=(kc == KC - 1)),
                             r=[wgu[b].b, XeT[b].b], w=[pG[fb].b], signal=(kc == KC - 1))
                    for kc in range(KC):
                        p.op('pe', lambda e: e.matmul(pU[fb].t[:, 0:CAP], wgu[b].t[:, kc, DE + fc * 128:DE + (fc + 1) * 128], XeT[b].t[:, kc, :],
                                                      start=(kc == 0), stop=(kc == KC - 1)),
                             r=[wgu[b].b, XeT[b].b], w=[pU[fb].b], signal=(kc == KC - 1))
                    p.op('act', lambda e: e.activation(out=sg[fb].t[:], in_=pG[fb].t[:, 0:CAP], func=AF.Silu), r=[pG[fb].b], w=[sg[fb].b])
                    p.op('dve', lambda e: e.tensor_tensor(out=actT[b].t[:, fc, :], in0=pU[fb].t[:, 0:CAP], in1=sg[fb].t[:], op=ALU.mult),
                         r=[pU[fb].b, sg[fb].b], w=[actT[b].b])
                if ex + 1 < NE:
                    transG(ex + 1)
                for ct, (c0, rows) in enumerate(CTS):
                    yb_ = (ex * NCT + ct) % 2
                    for cg in range(4):
                        pb = cg % 2
                        for fc in range(DE // 128):
                            p.op('pe', lambda e: e.matmul(pY[pb].t[0:rows, :], actT[b].t[:, fc, c0:c0 + rows], wdn[b].t[:, fc, cg * 512:(cg + 1) * 512],
                                                          start=(fc == 0), stop=(fc == DE // 128 - 1)),
                                 r=[actT[b].b, wdn[b].b], w=[pY[pb].b], signal=(fc == DE // 128 - 1))
                        if cg % 2 == 0:
                            p.op('act', lambda e: e.activation(out=ye[yb_].t[0:rows, cg * 512:(cg + 1) * 512], in_=pY[pb].t[0:rows, :], func=AF.Copy),
                                 r=[pY[pb].b], w=[ye[yb_].b])
                        else:
                            p.op('dve', lambda e: e.tensor_copy(out=ye[yb_].t[0:rows, cg * 512:(cg + 1) * 512], in_=pY[pb].t[0:rows, :]),
                                 r=[pY[pb].b], w=[ye[yb_].b])
                    r0 = ex * CAP + c0
                    p.dma('sp', [(self.ysS[r0:r0 + rows, :], ye[yb_].t[0:rows, :])], r=[ye[yb_].b], w=[self.ys_e[yi % 4]])
                    yi += 1
                if ex + 2 < NE:
                    loadG(ex + 2)
            p.barrier()

    def phaseH(self):
        nc, p, I = self.nc, self.p, self.I
        NR = NE * CAP
        with ExitStack() as les:
            NBH = 4
            ht = [self.sb(les, "htH%d" % i, [128, D], F32) for i in range(NBH)]
            y1 = [self.sb(les, "y1H%d" % i, [128, D], BF16) for i in range(NBH)]
            y2 = [self.sb(les, "y2H%d" % i, [128, D], BF16) for i in range(NBH)]

            def loadH(tt):
                b = tt % NBH
                p.dma('sp', [(ht[b].t[:], self.h2S[tt * 128:(tt + 1) * 128, :])], r=[self.h2Sb], w=[ht[b].b])
                for (yy, k) in ((y1, 0), (y2, 1)):
                    p.dma_custom('pool', lambda e, _k: e.indirect_dma_start(
                        out=yy[b].t[:, :], out_offset=None, in_=self.ysS[:, :],
                        in_offset=bass.IndirectOffsetOnAxis(ap=self.dsti[k].t[:, tt:tt + 1], axis=0),
                        ), 1,
                        r=[self.dsti[k].b, self.ys_zero] + self.ys_e, w=[yy[b].b])
            for tt in range(NBH - 1):
                loadH(tt)
            for tt in range(NT):
                b = tt % NBH
                if tt + NBH - 1 < NT:
                    loadH(tt + NBH - 1)
                p.op('dve', lambda e: e.scalar_tensor_tensor(out=ht[b].t[:], in0=y1[b].t[:], scalar=self.gate[0].t[:, tt:tt + 1],
                                                             in1=ht[b].t[:], op0=ALU.mult, op1=ALU.add),
                     r=[y1[b].b, self.gate[0].b, ht[b].b], w=[ht[b].b])
                p.op('dve', lambda e: e.scalar_tensor_tensor(out=ht[b].t[:], in0=y2[b].t[:], scalar=self.gate[1].t[:, tt:tt + 1],
                                                             in1=ht[b].t[:], op0=ALU.mult, op1=ALU.add),
                     r=[y2[b].b, self.gate[1].b, ht[b].b], w=[ht[b].b])
                p.dma('sp', [(self.y[tt * 128:(tt + 1) * 128, :], ht[b].t[:])], r=[ht[b].b], w=[self.yb])

    def phaseB_attn(self):
        nc, p, I = self.nc, self.p, self.I
        es = self.es
        self.oT = self.sb(es, "oT", [128, 8, T], BF16)
        oT = self.oT
        xnT = self.xnT
        with ExitStack() as les:
            kTz = self.sb(les, "kTz", [128, 2, 2, T], BF16)
            v = self.sb(les, "v", [128, NT, 128], BF16)
            bias = self.sb(les, "bias", [128, NQH, 256], BF16)
            esink = self.sb(les, "esink", [128, NQH], F32)
            esc = self.sb(les, "esc", [128, 8], F32)
            qgs = self.sb(les, "qgs", [128, 1], F32)
            wq = [self.sb(les, "wq%d" % i, [128, KC, 128], BF16) for i in range(2)]
            qc = [self.sb(les, "qc%d" % i, [128, T + 128], BF16) for i in range(2)]
            sqb1 = self.sb(les, "sqb", [128, 512], BF16)
            sqb = [sqb1, sqb1]
            rr = [self.sb(les, "rr%d" % i, [128, 512], F32) for i in range(2)]
            pT = [self.sb(les, "pT%d" % i, [128, NT, 256], BF16) for i in range(2)]
            rden = self.sb(les, "rden", [128, 512], F32)
            psQ = [self.ps(les, "psQ%d" % i, [128, 512]) for i in range(2)]
            psS = self.ps(les, "psS", [128, 512])
            psSc = [self.ps(les, "psSc%d" % i, [128, 2, 256]) for i in range(3)]
            psO = self.ps(les, "psO", [128, 512])
            psD = self.ps(les, "psD", [128, 512])
            p.dma('pool', [(bias.t[:], I['biasT'].rearrange("p (h c) -> p h c", h=NQH))], w=[bias.b])
            mask = self.cst.t[:, CST['mask']:CST['mask'] + 256]
            p.op('dve', lambda e: e.tensor_tensor(out=bias.t[:], in0=bias.t[:], in1=mask.unsqueeze(1).to_broadcast([128, NQH, 256]),
                                                  op=ALU.add), r=[bias.b, self.cst.b], w=[bias.b])
            p.op('act', lambda e: e.activation(out=esink.t[:], in_=self.prow.t[:, PROW['sinks']:PROW['sinks'] + NQH], func=AF.Exp),
                 r=[self.prow.b], w=[esink.b])
            es2 = esink.t[:].rearrange("p (c two) -> p c two", two=2)
            p.op('dve', lambda e: e.tensor_copy(out=esc.t[0:64, :], in_=es2[0:64, :, 0]), r=[esink.b], w=[esc.b])
            p.op('dve', lambda e: e.tensor_copy(out=esc.t[64:128, :], in_=es2[64:128, :, 1]), r=[esink.b], w=[esc.b])
            p.op('dve', lambda e: e.tensor_scalar(out=qgs.t[:], in0=self.pc('qg'), scalar1=HD ** -0.5, scalar2=None, op0=ALU.mult),
                 r=[self.pcol.b], w=[qgs.b])
            p.op('dve', lambda e: e.memset(qc[0].t[:, T:T + 128], 0.0), w=[qc[0].b])
            p.op('dve', lambda e: e.memset(qc[1].t[:, T:T + 128], 0.0), w=[qc[1].b])
            self.load_w(wq[0].t[:], wq[0].b, I['w_in'][:, OFF_V:OFF_V + 128])
            for g4 in range(4):
                pq = psQ[g4 % 2]
                for ti in range(4):
                    tt = g4 * 4 + ti
                    for kc in range(KC):
                        p.op('pe', lambda e: e.matmul(pq.t[:, ti * 128:(ti + 1) * 128], xnT.t[:, kc, tt * 128:(tt + 1) * 128],
                                                      wq[0].t[:, kc, :], start=(kc == 0), stop=(kc == KC - 1)),
                             r=[wq[0].b, xnT.b], w=[pq.b], signal=(kc == KC - 1))
                p.op('act', lambda e: e.activation(out=v.t[:, g4 * 4:(g4 + 1) * 4, :], in_=pq.t[:].rearrange("p (a c) -> p a c", a=4),
                                                   func=AF.Copy), r=[pq.b], w=[v.b])
            for kvh in range(NKV):
                wk = wq[(kvh + 1) % 2]
                src = I['w_in'][:, OFF_K + kvh * HD:OFF_K + (kvh + 1) * HD].rearrange("(k p) c -> p k c", p=128)
                p.dma('pool', [(wk.t[:, :, 0:HD], src), (wk.t[:, :, HD:2 * HD], src)], w=[wk.b])
                self.precast_next(after=psQ[1].b)
                self.qk_chunk(wk, kTz.t[:, kvh, 0, :], kTz.b, self.pc('kg'), self.pcol.b, psQ, psS, sqb, rr)
                p.op('dve', lambda e: e.tensor_copy(out=kTz.t[64:128, kvh, 1, :], in_=kTz.t[64:128, kvh, 0, :]), r=[kTz.b], w=[kTz.b])
                p.op('dve', lambda e: e.memset(kTz.t[64:128, kvh, 0, :], 0.0), w=[kTz.b])
                p.op('dve', lambda e: e.memset(kTz.t[0:64, kvh, 1, :], 0.0), w=[kTz.b])
            def loadQ(c):
                b = c % 2
                self.load_w(wq[b].t[:], wq[b].b, I['w_in'][:, OFF_Q + c * 128:OFF_Q + (c + 1) * 128])
                self.precast_next(after=psO.b)
                self.zero_fill_next(after=psO.b)

            def qpart(c, n):
                b = c % 2
                self.qk_part(wq[b], qc[b].t, qc[b].b, qgs.t[:], qgs.b, psQ, psS, sqb, rr, n)
            loadQ(0)
            for n in range(4):
                qpart(0, n)
            for c in range(8):
                b = c % 2
                kvh = c // 4
                if c + 1 < 8:
                    loadQ(c + 1)
                for hp in range(2):
                    h = 2 * c + hp
                    pr = slice(hp * 64, (hp + 1) * 64)
                    for jp in range(8):
                        if c + 1 < 8 and jp % 4 == 0:
                            qpart(c + 1, hp * 2 + jp // 4)
                        sc = psSc[jp % 3]
                        for jj in range(2):
                            j = 2 * jp + jj
                            p.op('pe', lambda e: e.matmul(sc.t[:, jj, :], kTz.t[:, kvh, hp, j * 128:(j + 1) * 128],
                                                          qc[b].t[:, j * 128:j * 128 + 256], start=True, stop=False),
                                 r=[kTz.b, qc[b].b], w=[sc.b], signal=False)
                            p.op('pe', lambda e: e.matmul(sc.t[:, jj, :], self.ident_bf.t[:], bias.t[:, h, :], start=False, stop=True),
                                 r=[self.ident_bf.b, bias.b], w=[sc.b], signal=(jj == 1))
                        p.op('act', lambda e: e.activation(out=pT[hp].t[:, 2 * jp:2 * jp + 2, :], in_=sc.t[:], func=AF.Exp),
                             r=[sc.b], w=[pT[hp].b])
                for g4 in range(4):
                    for hp in range(2):
                        pr = slice(hp * 64, (hp + 1) * 64)
                        vs = slice(kvh * HD, (kvh + 1) * HD)
                        for (pso, get_l) in ((psO, lambda n: v.t[:, n, vs]), (psD, lambda n: self.ones_bf.t[:, 0:HD])):
                            for nn in range(4):
                                n = g4 * 4 + nn
                                cs = slice(nn * 128, (nn + 1) * 128)
                                if n > 0:
                                    p.op('pe', lambda e: e.matmul(pso.t[pr, cs], get_l(n - 1), pT[hp].t[:, n - 1, 128:256],
                                                                  start=True, stop=False),
                                         r=[v.b, self.ones_bf.b, pT[hp].b], w=[pso.b], signal=False)
                                p.op('pe', lambda e: e.matmul(pso.t[pr, cs], get_l(n), pT[hp].t[:, n, 0:128],
                                                              start=(n == 0), stop=True),
                                     r=[v.b, self.ones_bf.b, pT[hp].b], w=[pso.b], signal=(nn == 3))
                    p.op('dve', lambda e: e.tensor_scalar(out=rden.t[:], in0=psD.t[:], scalar1=esc.t[:, c:c + 1], scalar2=None, op0=ALU.add),
                         r=[psD.b, esc.b], w=[rden.b])
                    p.op('dve', lambda e: e.reciprocal(out=rden.t[:], in_=rden.t[:]), r=[rden.b], w=[rden.b])
                    p.op('dve', lambda e: e.tensor_tensor(out=oT.t[:, c, g4 * 512:(g4 + 1) * 512], in0=psO.t[:], in1=rden.t[:], op=ALU.mult),
                         r=[psO.b, rden.b], w=[oT.b])
            p.barrier()
        self.dump('oT', oT, [128, 8, T], BF16)


PCOL = {}
PROW = {}
CST = {}


def _mk_layout():
    o = 0
    for name, n in [('gmix', KC), ('gx', KC), ('gmem', KC), ('gmoe', KC), ('dww', 8 * CW), ('dwb', 8), ('lng', 8),
                    ('lnb', 8), ('qg', 1), ('kg', 1), ('xkg', 1)]:
        PCOL[name] = o
        o += n
    global PCOL_N
    PCOL_N = o
    o = 0
    for name, n in [('sinks', NQH), ('xqg', XW), ('rtb', 36)]:
        PROW[name] = o
        o += n
    global PROW_N
    PROW_N = o
    o = 0
    for name, n in [('ident', 128), ('blk', 128), ('tri', 128), ('mask', 256), ('eps', 1), ('iota32', NE)]:
        CST[name] = o
        o += n
    global CST_N
    CST_N = o


PCOL_N = PROW_N = CST_N = 0
_mk_layout()


def _t5_bucket_np(dist):
    n = np.maximum(dist, 0)
    max_exact = 16
    large = max_exact + (np.log(np.maximum(n, 1).astype(np.float32) / max_exact)
                         / np.float32(np.log(128 / max_exact)) * (32 - max_exact)).astype(np.int32)
    large = np.minimum(large, 31)
    return np.where(n < max_exact, n, large)


def host_pack(inp):
    f = np.float32
    colT = lambda v: np.ascontiguousarray(v.reshape(-1, 128).T)
    pcol = np.zeros((128, PCOL_N), f)

    def put(name, arr):
        pcol[:, PCOL[name]:PCOL[name] + arr.shape[1]] = arr

    put('gmix', colT(inp['norm_mix_g'][0]))
    put('gx', colT(inp['norm_x_g'][0]))
    put('gmem', colT(inp['norm_mem_g'][0]))
    put('gmoe', colT(inp['norm_moe_g'][0]))
    dw = inp['conv_dw_w'][0][:, 0, :]
    put('dww', np.ascontiguousarray(dw.reshape(CW, 8, 128).transpose(2, 1, 0).reshape(128, 8 * CW)))
    put('dwb', colT(inp['conv_dw_b'][0]))
    put('lng', colT(inp['conv_ln_g'][0]))
    put('lnb', colT(inp['conv_ln_b'][0]))
    put('qg', np.tile(inp['q_norm_g'][0], 2).reshape(128, 1))
    put('kg', np.tile(inp['k_norm_g'][0], 2).reshape(128, 1))
    put('xkg', inp['xk_norm_g'][0].reshape(128, 1))
    prow = np.zeros((128, PROW_N), f)

    def putr(name, vec):
        prow[:, PROW[name]:PROW[name] + vec.shape[0]] = np.broadcast_to(vec[None, :], (128, vec.shape[0]))

    putr('sinks', inp['attn_sinks'][0])
    putr('xqg', np.tile(inp['xq_norm_g'][0], XH))
    putr('rtb', np.concatenate([inp['b_router_group'][0], inp['b_router_expert'][0]]))
    cst = np.zeros((128, CST_N), f)
    cst[:, CST['ident']:CST['ident'] + 128] = np.eye(128, dtype=f)
    blk = np.zeros((128, 128), f)
    blk[:64, :64] = 1
    blk[64:, 64:] = 1
    cst[:, CST['blk']:CST['blk'] + 128] = blk
    cst[:, CST['tri']:CST['tri'] + 128] = np.triu(np.ones((128, 128), f), 1)
    kj = np.arange(128)[:, None]
    qi = np.arange(128)[None, :]
    mask = np.zeros((128, 256), f)
    mask[:, :128] = np.where(qi - kj >= 0, 0.0, NEGM)
    mask[:, 128:] = np.where(kj > qi, 0.0, NEGM)
    cst[:, CST['mask']:CST['mask'] + 256] = mask
    cst[:, CST['eps']] = EPS
    cst[:, CST['iota32']:CST['iota32'] + NE] = np.arange(NE, dtype=f)[None, :]
    dist = np.concatenate([np.maximum(qi - kj, 0), np.clip(qi + 128 - kj, 0, 127)], axis=1)
    bk = _t5_bucket_np(dist)
    biasT = np.ascontiguousarray(inp['rel_bias'][bk].transpose(0, 2, 1)).reshape(128, NQH * 256).astype(f)
    w_rt = np.ascontiguousarray(np.concatenate([inp['w_router_group'][0], inp['w_router_expert'][0]], axis=1))
    shared = {
        'w_in': inp['w_in'][0], 'w_conv_out': inp['w_conv_out'][0], 'w_attn_out': inp['w_attn_out'][0],
        'w_mix_out': inp['w_mix_out'][0], 'w_xq': inp['w_xq'][0], 'w_xkv': inp['w_xkv'][0], 'w_xo': inp['w_xo'][0],
        'w_gu': inp['w_expert_gu'][0].reshape(NE * D, 2 * DE), 'w_dn': inp['w_expert_down'][0].reshape(NE * DE, D),
        'w_rt': w_rt, 'zsrc': np.zeros((640, D // 2), np.float32), 'gmoe_row': np.ascontiguousarray(np.broadcast_to(inp['norm_moe_g'][0][None, :], (128, D))), 'pcol': pcol, 'prow': prow, 'biasT': biasT, 'cst': cst,
    }
    return shared


def kernel(**inp):
    inp = {k: np.asarray(v) for k, v in inp.items()}
    shared = host_pack(inp)
    B = inp['x'].shape[0]
    k = Kern()
    nc = k.build()
    in_maps = []
    for b in range(B):
        m = dict(shared)
        m['x'] = np.ascontiguousarray(inp['x'][b])
        m['mem'] = np.ascontiguousarray(inp['mem'][b])
        in_maps.append(m)
    res = run_bass_kernel_spmd(nc, in_maps, core_ids=list(range(B)))
    return np.stack([np.asarray(r["y"]) for r in res.results], axis=0).astype(np.float32)
```
